# Optimizing a Trainium2 kernel written in Bass

```python
import jax, jax.numpy as jnp
from jax import lax
import numpy as np

D_MODEL = 1024
BATCH = 4
SEQ = 4096
DEPTH = 1

D_MIX = D_MODEL
GDN_HEADS = 4
GDN_DK = 128
GDN_DV = 128
MLSTM_HEADS = 4
MLSTM_DK = 128
MLSTM_DV = 128
N_DIR = 2
CONV_K = 5
CHUNK = 64
PEER_HEADS = 8
PEER_NKEYS = 128
PEER_N = PEER_NKEYS * PEER_NKEYS
PEER_DKEY = 256
PEER_TOPK = 16
PEER_BLOCK = 128
EPS = 1e-6

GDN_QK = GDN_HEADS * GDN_DK
GDN_V = GDN_HEADS * GDN_DV
ML_QK = MLSTM_HEADS * MLSTM_DK
ML_V = MLSTM_HEADS * MLSTM_DV
SPLIT_SIZES = (GDN_QK, GDN_QK, GDN_V, GDN_V, N_DIR * GDN_HEADS, N_DIR * GDN_HEADS,
               ML_QK, ML_QK, ML_V, ML_V, N_DIR * MLSTM_HEADS, N_DIR * MLSTM_HEADS)
D_IN = sum(SPLIT_SIZES)

kernel_name = "hybrid_gdn_mlstm_peer_block"


def _rmsnorm(x, g):
    x32 = x.astype(jnp.float32)
    y = x32 * lax.rsqrt(jnp.mean(x32 * x32, axis=-1, keepdims=True) + EPS)
    return (y * g.astype(jnp.float32)).astype(x.dtype)


def _l2norm(x):
    return x * lax.rsqrt(jnp.sum(x * x, axis=-1, keepdims=True) + EPS)


def _to_heads(x, n_heads):
    b, s, _ = x.shape
    return x.astype(jnp.float32).reshape(b, s, n_heads, -1).transpose(0, 2, 1, 3)


def _chunk(x):
    b, h, s = x.shape[:3]
    return x.reshape(b, h, s // CHUNK, CHUNK, *x.shape[3:])


def _flip(x):
    return jnp.flip(x, axis=2)


def _conv_centred(x, w):
    c = x.shape[-1]
    rhs = w.astype(jnp.float32).T[:, None, :]
    return lax.conv_general_dilated(x, rhs, window_strides=(1,),
                                    padding=[(CONV_K // 2, CONV_K // 2)],
                                    dimension_numbers=("NWC", "WIO", "NWC"),
                                    feature_group_count=c)


def _gated_delta_rule(q, k, v, g, beta):
    b_, h_, s_, dk = q.shape
    dv = v.shape[-1]
    q, k, v, g, beta = (_chunk(a) for a in (q, k, v, g, beta))
    g = jnp.cumsum(g, axis=-1)
    incl = jnp.tril(jnp.ones((CHUNK, CHUNK), dtype=bool))
    strict = jnp.tril(jnp.ones((CHUNK, CHUNK), dtype=bool), k=-1)
    decay = jnp.exp(jnp.where(incl, g[..., :, None] - g[..., None, :], -jnp.inf))
    k_beta = k * beta[..., None]
    a_mat = jnp.where(strict, jnp.einsum("bhncd,bhnsd->bhncs", k_beta, k) * decay, 0.0)
    eye = jnp.eye(CHUNK, dtype=q.dtype)
    t_mat = lax.linalg.triangular_solve(eye + a_mat, jnp.broadcast_to(eye, a_mat.shape),
                                        left_side=True, lower=True)
    u = jnp.einsum("bhncs,bhnse->bhnce", t_mat, v * beta[..., None])
    w = jnp.einsum("bhncs,bhnsd->bhncd", t_mat, k_beta * jnp.exp(g)[..., None])
    qk = jnp.where(incl, jnp.einsum("bhncd,bhnsd->bhncs", q, k) * decay, 0.0)
    q_dec = q * jnp.exp(g)[..., None]
    k_dec = k * jnp.exp(g[..., -1:] - g)[..., None]
    g_tot = jnp.exp(g[..., -1])

    def step(state, inp):
        u_c, w_c, qk_c, qd_c, kd_c, gt_c = inp
        v_new = u_c - jnp.einsum("bhcd,bhde->bhce", w_c, state)
        o_c = (jnp.einsum("bhcd,bhde->bhce", qd_c, state)
               + jnp.einsum("bhcs,bhse->bhce", qk_c, v_new))
        state = state * gt_c[..., None, None] + jnp.einsum("bhcd,bhce->bhde", kd_c, v_new)
        return state, o_c

    xs = tuple(jnp.moveaxis(a, 2, 0) for a in (u, w, qk, q_dec, k_dec, g_tot))
    _, o = lax.scan(step, jnp.zeros((b_, h_, dk, dv), q.dtype), xs)
    return jnp.moveaxis(o, 0, 2).reshape(b_, h_, s_, dv)


def _mlstm(q, k, v, i_pre, f_pre):
    b_, h_, s_, dk = q.shape
    dv = v.shape[-1]
    q, k, v, i_pre, f_pre = (_chunk(a) for a in (q, k, v, i_pre, f_pre))
    incl = jnp.tril(jnp.ones((CHUNK, CHUNK), dtype=bool))
    bcum = jnp.cumsum(jax.nn.log_sigmoid(f_pre), axis=-1)
    d_log = jnp.where(incl, bcum[..., :, None] - bcum[..., None, :] + i_pre[..., None, :], -jnp.inf)
    m_intra = jnp.max(d_log, axis=-1)
    p = jnp.exp(d_log - m_intra[..., None]) * jnp.einsum("bhncd,bhnsd->bhncs", q, k)
    num_intra = jnp.einsum("bhncs,bhnse->bhnce", p, v)
    den_intra = jnp.sum(p, axis=-1)
    b_last = bcum[..., -1]
    w_log = b_last[..., None] - bcum + i_pre
    m_w = jnp.max(w_log, axis=-1)
    wgt = jnp.exp(w_log - m_w[..., None])
    dc = jnp.einsum("bhnc,bhncd,bhnce->bhnde", wgt, k, v)
    dn = jnp.einsum("bhnc,bhncd->bhnd", wgt, k)

    def step(carry, inp):
        c_st, n_st, m_st = carry
        q_c, b_c, mi_c, num_c, den_c, bl_c, mw_c, dc_c, dn_c = inp
        inter_log = b_c + m_st[..., None]
        m_t = jnp.maximum(inter_log, mi_c)
        s_inter = jnp.exp(inter_log - m_t)
        s_intra = jnp.exp(mi_c - m_t)
        num = (s_inter[..., None] * jnp.einsum("bhcd,bhde->bhce", q_c, c_st)
               + s_intra[..., None] * num_c)
        den = s_inter * jnp.einsum("bhcd,bhd->bhc", q_c, n_st) + s_intra * den_c
        h = num / jnp.maximum(jnp.abs(den), jnp.exp(-m_t))[..., None]
        m_new = jnp.maximum(bl_c + m_st, mw_c)
        s_old = jnp.exp(bl_c + m_st - m_new)
        s_new = jnp.exp(mw_c - m_new)
        c_st = s_old[..., None, None] * c_st + s_new[..., None, None] * dc_c
        n_st = s_old[..., None] * n_st + s_new[..., None] * dn_c
        return (c_st, n_st, m_new), h

    xs = tuple(jnp.moveaxis(a, 2, 0) for a in
               (q, bcum, m_intra, num_intra, den_intra, b_last, m_w, dc, dn))
    init = (jnp.zeros((b_, h_, dk, dv), q.dtype), jnp.zeros((b_, h_, dk), q.dtype),
            jnp.zeros((b_, h_), q.dtype))
    _, h = lax.scan(step, init, xs)
    return jnp.moveaxis(h, 0, 2).reshape(b_, h_, s_, dv)


def _gdn_group(q, k, v, z, a, bb, conv_w, a_log, dt_bias, norm_g):
    bsz, s, _ = q.shape
    qkv = jax.nn.silu(_conv_centred(jnp.concatenate([q, k, v], axis=-1).astype(jnp.float32), conv_w))
    qh = _l2norm(_to_heads(qkv[..., :GDN_QK], GDN_HEADS)) * (GDN_DK ** -0.5)
    kh = _l2norm(_to_heads(qkv[..., GDN_QK:2 * GDN_QK], GDN_HEADS))
    vh = _to_heads(qkv[..., 2 * GDN_QK:], GDN_HEADS)
    a = a.astype(jnp.float32).reshape(bsz, s, N_DIR, GDN_HEADS)
    bb = bb.astype(jnp.float32).reshape(bsz, s, N_DIR, GDN_HEADS)
    g = -jnp.exp(a_log.astype(jnp.float32)) * jax.nn.softplus(a + dt_bias.astype(jnp.float32))
    beta = jax.nn.sigmoid(bb)
    g = g.transpose(2, 0, 3, 1)
    beta = beta.transpose(2, 0, 3, 1)
    o_f = _gated_delta_rule(qh, kh, vh, g[0], beta[0])
    o_b = _flip(_gated_delta_rule(_flip(qh), _flip(kh), _flip(vh), _flip(g[1]), _flip(beta[1])))
    o = (o_f + o_b).transpose(0, 2, 1, 3)
    o = o * lax.rsqrt(jnp.mean(o * o, axis=-1, keepdims=True) + EPS) * norm_g.astype(jnp.float32)
    return o.reshape(bsz, s, GDN_V) * jax.nn.silu(z.astype(jnp.float32))


def _mlstm_group(q, k, v, o_gate, i_in, f_in, i_bias, f_bias, norm_g):
    bsz, s, _ = q.shape
    qh = _to_heads(q, MLSTM_HEADS)
    kh = _to_heads(k, MLSTM_HEADS) * (MLSTM_DK ** -0.5)
    vh = _to_heads(v, MLSTM_HEADS)
    i_pre = (i_in.astype(jnp.float32).reshape(bsz, s, N_DIR, MLSTM_HEADS)
             + i_bias.astype(jnp.float32)).transpose(2, 0, 3, 1)
    f_pre = (f_in.astype(jnp.float32).reshape(bsz, s, N_DIR, MLSTM_HEADS)
             + f_bias.astype(jnp.float32)).transpose(2, 0, 3, 1)
    h_f = _mlstm(qh, kh, vh, i_pre[0], f_pre[0])
    h_b = _flip(_mlstm(_flip(qh), _flip(kh), _flip(vh), _flip(i_pre[1]), _flip(f_pre[1])))
    h = (h_f + h_b).transpose(0, 2, 1, 3)
    h = (h * lax.rsqrt(jnp.mean(h * h, axis=-1, keepdims=True) + EPS)).reshape(bsz, s, ML_V)
    return h * norm_g.astype(jnp.float32) * jax.nn.sigmoid(o_gate.astype(jnp.float32))


def _peer(h, w_q, sub_keys, u_tab, v_tab):
    bsz, s, d = h.shape
    t = h.reshape(-1, d)
    n_tok = t.shape[0]
    qry = (t @ w_q).astype(jnp.float32).reshape(n_tok, PEER_HEADS, 2, PEER_DKEY // 2)
    scores = jnp.einsum("thpd,hpkd->thpk", qry, sub_keys.astype(jnp.float32))
    s_top, i_top = lax.top_k(scores, PEER_TOPK)
    cand = (s_top[:, :, 0, :, None] + s_top[:, :, 1, None, :]).reshape(n_tok, PEER_HEADS, -1)
    cand_idx = (i_top[:, :, 0, :, None] * PEER_NKEYS + i_top[:, :, 1, None, :]).reshape(n_tok, PEER_HEADS, -1)
    best, pos = lax.top_k(cand, PEER_TOPK)
    idx = jnp.take_along_axis(cand_idx, pos, axis=-1)
    gate = jax.nn.softmax(best, axis=-1)
    nb = n_tok // PEER_BLOCK

    def block_fn(args):
        tb, ib, gb = args
        u = u_tab[ib]
        act = jax.nn.gelu(jnp.einsum("td,thkd->thk", tb, u).astype(jnp.float32), approximate=False)
        vv = v_tab[ib]
        return jnp.einsum("thk,thkd->td", (gb * act).astype(vv.dtype), vv)

    out = lax.map(block_fn, (t.reshape(nb, PEER_BLOCK, d),
                             idx.reshape(nb, PEER_BLOCK, PEER_HEADS, PEER_TOPK),
                             gate.reshape(nb, PEER_BLOCK, PEER_HEADS, PEER_TOPK)))
    return out.reshape(bsz, s, d).astype(h.dtype)


def setup_inputs(seed: int = 0) -> dict:
    key = jax.random.key(seed)
    ks = jax.random.split(key, 20)
    f32 = jnp.float32
    nrm = lambda k, shp, sc: jax.random.normal(k, shp, f32) * sc
    dt = jnp.exp(jax.random.uniform(ks[5], (DEPTH, N_DIR, GDN_HEADS), f32,
                                    np.log(1e-3).astype(np.float32), np.log(1e-1).astype(np.float32)))
    return {
        "x": nrm(ks[0], (BATCH, SEQ, D_MODEL), 1.0),
        "norm1_g": 1.0 + nrm(ks[1], (DEPTH, D_MODEL), 0.02),
        "w_in": nrm(ks[2], (DEPTH, D_MODEL, D_IN), D_MODEL ** -0.5),
        "conv_w": nrm(ks[3], (DEPTH, 2 * GDN_QK + GDN_V, CONV_K), CONV_K ** -0.5),
        "gdn_a_log": jnp.log(jax.random.uniform(ks[4], (DEPTH, N_DIR, GDN_HEADS), f32, 1.0, 16.0)),
        "gdn_dt_bias": jnp.log(jnp.expm1(dt)),
        "gdn_norm_g": 1.0 + nrm(ks[6], (DEPTH, GDN_DV), 0.02),
        "mlstm_i_bias": nrm(ks[7], (DEPTH, N_DIR, MLSTM_HEADS), 0.1),
        "mlstm_f_bias": jax.random.uniform(ks[8], (DEPTH, N_DIR, MLSTM_HEADS), f32, 3.0, 6.0),
        "mlstm_norm_g": 1.0 + nrm(ks[9], (DEPTH, ML_V), 0.02),
        "w_out": nrm(ks[10], (DEPTH, D_MIX, D_MODEL), D_MIX ** -0.5),
        "norm2_g": 1.0 + nrm(ks[11], (DEPTH, D_MODEL), 0.02),
        "peer_wq": nrm(ks[12], (DEPTH, D_MODEL, PEER_HEADS * PEER_DKEY), D_MODEL ** -0.5),
        "peer_keys": nrm(ks[13], (DEPTH, PEER_HEADS, 2, PEER_NKEYS, PEER_DKEY // 2), (PEER_DKEY // 2) ** -0.5),
        "peer_u": nrm(ks[14], (DEPTH, PEER_N, D_MODEL), D_MODEL ** -0.5),
        "peer_v": nrm(ks[15], (DEPTH, PEER_N, D_MODEL), PEER_HEADS ** -0.5),
        "normf_g": 1.0 + nrm(ks[16], (D_MODEL,), 0.02),
    }


def reference(x, norm1_g, w_in, conv_w, gdn_a_log, gdn_dt_bias, gdn_norm_g, mlstm_i_bias,
              mlstm_f_bias, mlstm_norm_g, w_out, norm2_g, peer_wq, peer_keys, peer_u, peer_v,
              normf_g):
    offsets = [int(o) for o in np.cumsum(SPLIT_SIZES)[:-1]]
    for l in range(DEPTH):
        h = _rmsnorm(x, norm1_g[l])
        p = h @ w_in[l]
        (gq, gk, gv, gz, ga, gb, mq, mk, mv, mo, mi, mf) = jnp.split(p, offsets, axis=-1)
        y_a = _gdn_group(gq, gk, gv, gz, ga, gb, conv_w[l], gdn_a_log[l], gdn_dt_bias[l], gdn_norm_g[l])
        y_b = _mlstm_group(mq, mk, mv, mo, mi, mf, mlstm_i_bias[l], mlstm_f_bias[l], mlstm_norm_g[l])
        y = jnp.concatenate([y_a, y_b], axis=-1).astype(x.dtype)
        x = x + y @ w_out[l]
        h2 = _rmsnorm(x, norm2_g[l])
        x = x + _peer(h2, peer_wq[l], peer_keys[l], peer_u[l], peer_v[l])
    return _rmsnorm(x, normf_g)
```

```python
import numpy as np
from contextlib import ExitStack
import concourse.bass as bass
import concourse.mybir as mybir

F32 = mybir.dt.float32; BF16 = mybir.dt.bfloat16; U32 = mybir.dt.uint32; I32 = mybir.dt.int32
AF = mybir.ActivationFunctionType; ALU = mybir.AluOpType; AX = mybir.AxisListType

class Buf:
    __slots__ = ("name", "w", "r", "dsem", "dcnt", "t")
    def __init__(self, name, t=None):
        self.name = name; self.w = None; self.r = {}; self.dsem = None; self.dcnt = 0; self.t = t
    def __getitem__(self, k):
        return self.t[k]

ENGS = ("pe", "dve", "act", "pool", "sp")

class Prog:
    def __init__(self, nc, same_sync=("dve", "act", "pool")):
        self.nc = nc
        self.es = ExitStack()
        self.sem = {}
        for e in ENGS:
            self.sem[e] = self.es.enter_context(nc.semaphore("prog_" + e))
        self.count = {e: 0 for e in ENGS}
        self.waited = {e: {} for e in ENGS}
        self.prog = {e: [] for e in ENGS}
        self.same_sync = set(same_sync)
        self.nbuf = 0
        self.ndsem = 0

    def _arena(self):
        if getattr(self, "arena", None) is None:
            nbytes = (self.nc.sbuf_bytes_remaining - 2048) // 64 * 64
            self.arena_words = nbytes // 4
            self.arena = self.es.enter_context(self.nc.sbuf_tensor("arena", [128, self.arena_words], F32))
            self.aoff = 0
            self.dbufs = []
        return self.arena

    def sb(self, name, shape, dt):
        ar = self._arena()
        shape = list(shape)
        assert shape[0] == 128
        nelem = 1
        for s_ in shape[1:]:
            nelem *= s_
        esz = {F32: 4, BF16: 2, U32: 4, I32: 4}[dt]
        nwords = (nelem * esz + 63) // 64 * 16
        assert self.aoff + nwords <= self.arena_words, ("SBUF arena overflow", name, self.aoff * 4, nwords * 4)
        ap = ar[:, self.aoff:self.aoff + nwords]
        self.aoff += nwords
        if dt != F32:
            ap = ap.bitcast(dt)
        ap = ap[:, 0:nelem]
        if len(shape) == 3:
            ap = ap.rearrange("p (a b) -> p a b", a=shape[1])
        elif len(shape) == 4:
            ap = ap.rearrange("p (a b c) -> p a b c", a=shape[1], b=shape[2])
        return Buf(name, ap)

    def mark(self):
        self._arena()
        return self.aoff

    def release(self, mark):
        self.aoff = mark

    def ps(self, name, shape, dt=F32):
        t = self.es.enter_context(self.nc.psum_tensor("p_" + name, list(shape), dt))
        return Buf(name, t)
    def view(self, name, t):
        return Buf(name, t)

    def _need(self, eng, waits, ev):
        if ev is None:
            return
        key, val = ev
        if key == eng and eng not in self.same_sync:
            return
        if self.waited[eng].get(key, 0) >= val:
            return
        if waits.get(key, 0) < val:
            waits[key] = val

    def _deps(self, eng, reads, writes):
        waits = {}
        for b in reads:
            self._need(eng, waits, b.w)
        for b in writes:
            self._need(eng, waits, b.w)
            for s, v in b.r.items():
                self._need(eng, waits, (s, v))
        for s, v in waits.items():
            self.waited[eng][s] = v
        return waits

    def _mark(self, ev, reads, writes):
        s, v = ev
        for b in reads:
            if b.r.get(s, 0) < v:
                b.r[s] = v
        for b in writes:
            b.w = ev
            b.r = {}

    def op(self, eng, fn, reads=(), writes=()):
        waits = self._deps(eng, reads, writes)
        self.count[eng] += 1
        ev = (eng, self.count[eng])
        self._mark(ev, reads, writes)
        self.prog[eng].append((waits, fn, eng, self.count[eng]))
        return ev

    def dma(self, eng, fn, dbuf, reads=(), writes=()):
        waits = self._deps(eng, reads, writes)
        if dbuf.dsem is None:
            dbuf.dsem = self.es.enter_context(self.nc.semaphore("d_" + dbuf.name))
            self.ndsem += 1
            self._arena()
            self.dbufs.append(dbuf)
        dbuf.dcnt += 16
        ev = (dbuf.dsem, dbuf.dcnt)
        self._mark(ev, reads, writes)
        self.prog[eng].append((waits, fn, dbuf.dsem, 16))
        return ev

    def wait_all(self, eng, bufs):
        waits = {}
        for b in bufs:
            self._need(eng, waits, b.w)
            for s, v in b.r.items():
                self._need(eng, waits, (s, v))
        for s, v in waits.items():
            self.waited[eng][s] = v
        self.prog[eng].append((waits, None, None, 0))

    def barrier(self):
        evs = [(e, self.count[e]) for e in ENGS if self.count[e] > 0]
        evs += [(b.dsem, b.dcnt) for b in self.dbufs]
        for e in ENGS:
            waits = {}
            for ev in evs:
                self._need(e, waits, ev)
            for s_, v in waits.items():
                self.waited[e][s_] = v
            self.prog[e].append((waits, None, None, 0))

    def emit(self):
        nc = self.nc
        sig = {e: set() for e in ENGS}
        for e in ENGS:
            for waits, fn, key, val in self.prog[e]:
                for k, v in waits.items():
                    if isinstance(k, str):
                        sig[k].add(v)
        rank = {}
        for e in ENGS:
            rank[e] = {idx: i + 1 for i, idx in enumerate(sorted(sig[e]))}
        self.nsig = {e: len(sig[e]) for e in ENGS}
        with nc.Block() as block:
            def run(e, engine):
                for waits, fn, key, val in self.prog[e]:
                    for k, v in waits.items():
                        if isinstance(k, str):
                            engine.wait_ge(self.sem[k], rank[k][v])
                        else:
                            engine.wait_ge(k, v)
                    if fn is not None:
                        ins = fn(engine)
                        if isinstance(key, str):
                            if val in rank[key]:
                                ins.then_inc(self.sem[key], 1)
                        else:
                            ins.then_inc(key, val)
            @block.tensor
            def _(engine): run("pe", engine)
            @block.vector
            def _(engine): run("dve", engine)
            @block.scalar
            def _(engine): run("act", engine)
            @block.gpsimd
            def _(engine): run("pool", engine)
            @block.sync
            def _(engine): run("sp", engine)
        self.es.close()


def _bufs(*vs):
    out = []
    for v in vs:
        if isinstance(v, tuple):
            out.append(v[0])
    return out

class Ops:
    def __init__(self, P):
        self.P = P

    def mm(self, out, lhsT, rhs, start=True, stop=True):
        o, l, r = out[1], lhsT[1], rhs[1]
        self.P.op("pe", lambda e: e.matmul(o, lhsT=l, rhs=r, start=start, stop=stop),
                  reads=_bufs(lhsT, rhs), writes=_bufs(out))

    def tr(self, out, in_, ident):
        o, i, d = out[1], in_[1], ident[1]
        self.P.op("pe", lambda e: e.transpose(o, i, d), reads=_bufs(in_, ident), writes=_bufs(out))

    def act(self, out, in_, func, scale=1.0, bias=None, accum=None):
        o, i = out[1], in_[1]
        kw = {}
        rd = _bufs(in_)
        wr = _bufs(out)
        if bias is not None:
            if isinstance(bias, tuple):
                kw["bias"] = bias[1]; rd += [bias[0]]
            else:
                kw["bias"] = bias
        if isinstance(scale, tuple):
            kw["scale"] = scale[1]; rd += [scale[0]]
        else:
            kw["scale"] = scale
        if accum is not None:
            kw["accum_out"] = accum[1]; wr += [accum[0]]
        self.P.op("act", lambda e: e.activation(out=o, in_=i, func=func, **kw), reads=rd, writes=wr)

    def cp(self, out, in_, eng="act"):
        o, i = out[1], in_[1]
        if eng == "act":
            self.P.op("act", lambda e: e.copy(out=o, in_=i), reads=_bufs(in_), writes=_bufs(out))
        else:
            self.P.op(eng, lambda e: e.tensor_copy(out=o, in_=i), reads=_bufs(in_), writes=_bufs(out))

    def tt(self, out, a, b, op, eng="dve"):
        o, x, y = out[1], a[1], b[1]
        self.P.op(eng, lambda e: e.tensor_tensor(out=o, in0=x, in1=y, op=op), reads=_bufs(a, b), writes=_bufs(out))

    def ts(self, out, a, s1, s2=None, op0=ALU.mult, op1=ALU.bypass, eng="dve", accum=None):
        o, x = out[1], a[1]
        rd = _bufs(a, s1, s2)
        wr = _bufs(out)
        v1 = s1[1] if isinstance(s1, tuple) else s1
        v2 = s2[1] if isinstance(s2, tuple) else s2
        kw = {}
        if accum is not None:
            kw["accum_out"] = accum[1]; wr += [accum[0]]
        self.P.op(eng, lambda e: e.tensor_scalar(out=o, in0=x, scalar1=v1, scalar2=v2, op0=op0, op1=op1, **kw), reads=rd, writes=wr)

    def stt(self, out, a, scalar, b, op0, op1, accum=None):
        o, x, y = out[1], a[1], b[1]
        rd = _bufs(a, b, scalar)
        wr = _bufs(out)
        sv = scalar[1] if isinstance(scalar, tuple) else scalar
        kw = {}
        if accum is not None:
            kw["accum_out"] = accum[1]; wr += [accum[0]]
        self.P.op("dve", lambda e: e.scalar_tensor_tensor(out=o, in0=x, scalar=sv, in1=y, op0=op0, op1=op1, **kw), reads=rd, writes=wr)

    def red(self, out, in_, op, axis=AX.X, eng="dve"):
        o, i = out[1], in_[1]
        self.P.op(eng, lambda e: e.tensor_reduce(out=o, in_=i, axis=axis, op=op), reads=_bufs(in_), writes=_bufs(out))

    def recip(self, out, in_):
        o, i = out[1], in_[1]
        self.P.op("dve", lambda e: e.reciprocal(out=o, in_=i), reads=_bufs(in_), writes=_bufs(out))

    def memset(self, out, val, eng="dve"):
        o = out[1]
        self.P.op(eng, lambda e: e.memset(o, val), writes=_bufs(out))

    def dma(self, out, in_, eng="sp", dbuf=None):
        o = out[1] if isinstance(out, tuple) else out
        i = in_[1] if isinstance(in_, tuple) else in_
        rd = _bufs(in_); wr = _bufs(out)
        db = dbuf if dbuf is not None else (wr[0] if wr else rd[0])
        self.P.dma(eng, lambda e: e.dma_start(out=o, in_=i), db, reads=rd, writes=wr)


import numpy as np
import concourse.bass as bass
import concourse.mybir as mybir

NEG = -30000.0
EPS = 1e-6


def bc_last(ap, n):
    return ap.unsqueeze(2).to_broadcast([ap.shape[0], ap.shape[1], n])


def bc_mid(ap, n):
    return ap.unsqueeze(1).to_broadcast([ap.shape[0], n, ap.shape[1]])


def build_consts():
    i = np.arange(128)
    blk = (i[:, None] // 64) == (i[None, :] // 64)
    LT = ((i[None, :] <= i[:, None]) & blk).astype(np.float32)
    UT = ((i[None, :] >= i[:, None]) & blk).astype(np.float32)
    SL = ((i[None, :] < i[:, None]) & blk).astype(np.float32)
    SU = ((i[None, :] > i[:, None]) & blk).astype(np.float32)
    ident = np.eye(128, dtype=np.float32)
    ones = np.ones((128, 128), np.float32)
    blk0 = np.repeat((i < 64).astype(np.float32)[:, None], 128, 1)
    blk1 = np.repeat((i >= 64).astype(np.float32)[:, None], 128, 1)
    mats = dict(LT=LT, UT=UT, SL=SL, SU=SU, ident=ident, ones=ones, blk0=blk0, blk1=blk1,
                NSL=NEG * (1 - SL), NSU=NEG * (1 - SU), NUT=NEG * (1 - UT), NLT=NEG * (1 - LT))
    names = list(mats.keys())
    arr = np.concatenate([mats[k] for k in names], axis=1).astype(np.float32)
    return names, arr


CONST_NAMES, CONST_ARR = build_consts()


def build(NT_OTHER, NT_OWN, phase2=False, debug=False):
    nc = bass.Bass("TRN2", target_bir_lowering=False)
    NT = NT_OTHER + NT_OWN
    S = 128 * NT
    S_OWN = 128 * NT_OWN

    def din(name, shape, dt=F32):
        return nc.dram_tensor(name, list(shape), dt, kind="ExternalInput").ap()

    xT_d = din("xT", [1024, S + 4])
    xown_d = din("x_own", [S_OWN, 1024])
    wfm_d = din("w_in_fm", [1024, 2048])
    wtm_d = din("w_in_tm", [1024, 2080])
    wout_d = din("w_out", [1024, 1024])
    consts_d = din("consts", [128, len(CONST_NAMES) * 128])
    convw_d = din("conv_w", [128, 60])
    n1g_d = din("norm1_g", [128, 8])
    gpadd_d = din("gp_add", [128, 32])
    alog_d = din("alog", [128, 8])
    gng_d = din("gdn_norm_g", [128, 128])
    mng_d = din("mlstm_norm_g", [128, 512])
    wq_d = din("wq", [1024, 2048])
    keysT_d = din("keysT", [128, 2048])
    n2g_d = din("n2g", [128, 1024])
    nfg_d = din("nfg", [128, 1024])
    UT_d = din("UT", [128, 128 * 1024])
    V_d = din("Vt", [128, 128 * 1024])
    iot_d = din("iotas", [128, 144])
    x1_d = nc.dram_tensor("x1_scr", [NT_OWN, 128, 1024], F32, kind="Internal").ap()
    UTs_d = nc.dram_tensor("UT_bf", [128, 128 * 1024], BF16, kind="Internal").ap()
    Vs_d = nc.dram_tensor("V_bf", [128, 128 * 1024], BF16, kind="Internal").ap()
    out_d = nc.dram_tensor("out", [S_OWN, 1024], F32, kind="ExternalOutput").ap()
    ob_d = nc.dram_tensor("ob_scr", [NT_OWN, 128, 1024], F32, kind="Internal").ap()

    P = Prog(nc)
    O = Ops(P)

    consts = P.sb("consts", [128, len(CONST_NAMES) * 128], F32)
    O.dma((consts, consts.t[:, :]), consts_d)
    C = {k: (consts, consts.t[:, i * 128:(i + 1) * 128]) for i, k in enumerate(CONST_NAMES)}
    identb = P.sb("identb", [128, 128], BF16)
    O.cp((identb, identb.t[:, :]), C["ident"], eng="dve")

    iot = P.sb("iot", [128, 144], F32)
    O.dma((iot, iot.t[:, :]), iot_d)
    PH_MARK = P.mark()
    wfm = P.sb("wfm", [128, 8, 2048], BF16)
    wtm = P.sb("wtm", [128, 8, 2080], BF16)
    wout = P.sb("wout", [128, 8, 1024], BF16)
    for j in range(8):
        O.dma((wfm, wfm.t[:, j, :]), wfm_d[j * 128:(j + 1) * 128, :], eng="pool")
        O.dma((wtm, wtm.t[:, j, :]), wtm_d[j * 128:(j + 1) * 128, :], eng="pool")
        O.dma((wout, wout.t[:, j, :]), wout_d[j * 128:(j + 1) * 128, :], eng="pool")
    UTs = P.view("UTs", UTs_d)
    Vs = P.view("Vs", Vs_d)
    CH = 8192

    def conv_chunk(cix):
        O.dma((UTs, UTs_d[:, cix * CH:(cix + 1) * CH]), UT_d[:, cix * CH:(cix + 1) * CH], eng="pool")
        O.dma((Vs, Vs_d[:, cix * CH:(cix + 1) * CH]), V_d[:, cix * CH:(cix + 1) * CH], eng="pool")
    n_conv = 131072 // CH
    convw = P.sb("convw", [128, 12, 5], F32)
    O.dma((convw, convw.t[:, :, :].rearrange("p c k -> p (c k)")), convw_d)
    n1g = P.sb("n1g", [128, 8], F32)
    O.dma((n1g, n1g.t[:, :]), n1g_d)
    gpadd = P.sb("gpadd", [128, 32], F32)
    O.dma((gpadd, gpadd.t[:, :]), gpadd_d)
    alog = P.sb("alog", [128, 8], F32)
    O.dma((alog, alog.t[:, :]), alog_d)
    for j in range(8):
        O.act((wfm, wfm.t[:, j, :]), (wfm, wfm.t[:, j, :]), AF.Copy, scale=(n1g, n1g.t[:, j:j + 1]))
        O.act((wtm, wtm.t[:, j, :]), (wtm, wtm.t[:, j, :]), AF.Copy, scale=(n1g, n1g.t[:, j:j + 1]))
    nEa = P.sb("nEa", [128, 8], F32)
    O.act((nEa, nEa.t[:, :]), (alog, alog.t[:, :]), AF.Exp)
    O.ts((nEa, nEa.t[:, :]), (nEa, nEa.t[:, :]), -1.0, op0=ALU.mult)
    gng = P.sb("gng", [128, 4, 128], F32)
    for h in range(4):
        O.dma((gng, gng.t[:, h, :]), gng_d)
    mng = P.sb("mng", [128, 4, 128], F32)
    O.dma((mng, mng.t[:, :, :].rearrange("p h d -> p (h d)")), mng_d)

    banks = [P.ps("bank%d" % i, [128, 512], F32) for i in range(8)]
    bank_i = [0]

    def pbank():
        b = banks[bank_i[0] % 8]
        bank_i[0] += 1
        return b

    def pv4(b):
        return b.t[:, :].rearrange("p (h s) -> p h s", h=4)

    xt = [P.sb("xt0", [128, 8, 132], F32)]
    rstd = P.sb("rstd", [128, 132], F32)
    hT = P.sb("hT", [128, 8, 132], BF16)
    pre = P.sb("pre", [128, 12, 132], F32)
    cacc = P.sb("cacc", [128, 12, 128], F32)
    ctmp = P.sb("ctmp", [128, 4, 128], F32)
    nsq = ctmp
    cbufs = [P.sb("cbuf%d" % i, [128, 4, 128], BF16) for i in range(2)]
    nrs = ctmp
    gqT2 = [P.sb("gqT%d" % i, [128, 4, 128], BF16) for i in range(2)]
    gkT2 = [P.sb("gkT%d" % i, [128, 4, 128], BF16) for i in range(2)]
    mqT2 = [P.sb("mqT%d" % i, [128, 4, 128], BF16) for i in range(2)]
    mkT2 = [P.sb("mkT%d" % i, [128, 4, 128], BF16) for i in range(2)]
    mvx3 = [P.sb("mvx%d" % i, [128, 4, 130], BF16) for i in range(3)]
    for i in range(3):
        O.memset((mvx3[i], mvx3[i].t[:, :, :]), 1.0)
    tot3 = [P.sb("tot%d" % i, [128, 16], F32) for i in range(3)]
    mkt2 = [P.sb("mkt%d" % i, [128, 4, 128], F32) for i in range(2)]
    ktok2 = [P.sb("ktok%d" % i, [128, 4, 128], F32) for i in range(2)]
    vtok2 = [P.sb("vtok%d" % i, [128, 4, 128], F32) for i in range(2)]
    gpre2 = [P.sb("gpre%d" % i, [128, 32], F32) for i in range(2)]
    gsm_2 = [P.sb("gsm%d" % i, [128, 16], F32) for i in range(2)]
    gcat2 = [P.sb("gcat%d" % i, [128, 8], F32) for i in range(2)]
    sm2_2 = [P.sb("sm2%d" % i, [128, 32], F32) for i in range(2)]
    SLg = P.sb("SLg", [128, 4, 128], F32)
    UTg = P.sb("UTg", [128, 4, 128], F32)
    Dm = P.sb("Dm", [128, 4, 128], F32)
    A0 = P.sb("A0", [128, 4, 128], F32)
    A0T = P.sb("A0T", [128, 4, 128], F32)
    Mb0 = P.sb("Mb0", [128, 4, 128], F32)
    MTb0 = P.sb("MTb0", [128, 4, 128], F32)
    Pm = P.sb("Pm", [128, 4, 128], F32)
    TTb = P.sb("TTb", [128, 4, 128], BF16)
    vb = P.sb("vb", [128, 4, 128], BF16)
    kbg = P.sb("kbg", [128, 4, 128], BF16)
    kdec_2 = [P.sb("kdec%d" % i, [128, 4, 128], BF16) for i in range(2)]
    u_sb_2 = [P.sb("u_sb%d" % i, [128, 4, 128], F32) for i in range(2)]
    wT_2 = [P.sb("wT%d" % i, [128, 4, 128], BF16) for i in range(2)]
    qkT_2 = [P.sb("qkT%d" % i, [128, 4, 128], BF16) for i in range(2)]
    qdT_2 = [P.sb("qdT%d" % i, [128, 4, 128], BF16) for i in range(2)]
    Eg = Dm
    vnew = P.sb("vnew", [128, 4, 128], BF16)
    kw_2 = [P.sb("kw%d" % i, [128, 4, 128], BF16) for i in range(2)]
    pT_2 = [P.sb("pT%d" % i, [128, 4, 128], BF16) for i in range(2)]
    mqd_2 = [P.sb("mqd%d" % i, [128, 4, 128], BF16) for i in range(2)]
    o_sb = P.sb("o_sb", [128, 8, 128], F32)
    ob_sb = P.sb("ob_sb", [128, 8, 128], F32)
    y_sb = P.sb("y_sb", [128, 8, 128], BF16)
    yT = P.sb("yT", [128, 8, 128], BF16)
    zs3 = [P.sb("zs%d" % i, [128, 8, 128], BF16) for i in range(3)]
    xtok = P.sb("xtok", [128, 1024], F32)
    x1 = xtok
    Sg = P.sb("Sg", [128, 4, 128], F32)
    Sgb = P.sb("Sgb", [128, 4, 128], BF16)
    Cm = P.sb("Cm", [128, 4, 130], F32)
    Cmb = P.sb("Cmb", [128, 4, 130], BF16)
    gsm2 = P.sb("gsm2", [128, 16], F32)
    negb = P.sb("negb", [128, 2, 128], BF16)
    negb_dir = [None]

    def reset_states():
        O.memset((Sg, Sg.t), 0.0)
        O.memset((Sgb, Sgb.t), 0.0)
        O.memset((Cm, Cm.t), 0.0)
        O.memset((Cmb, Cmb.t), 0.0)
    gpool = [banks[0], banks[1], banks[2]]
    mpool = [banks[4], banks[5], banks[6]]
    fpool = [banks[3], banks[7]]
    pst = {"g": 0, "m": 0, "f": 0, "gpin": set(), "mpin": set(), "fpin": set()}

    def falloc():
        return _alloc(fpool, "f", "fpin", False)

    def _alloc(pool, key, pinkey, pin):
        for _ in range(8):
            b = pool[pst[key] % len(pool)]
            pst[key] += 1
            if b.name not in pst[pinkey]:
                if pin:
                    pst[pinkey].add(b.name)
                return b
        raise RuntimeError("no free psum bank")

    def galloc(pin=False):
        return _alloc(gpool, "g", "gpin", pin)

    def malloc_(pin=False):
        return _alloc(mpool, "m", "mpin", pin)

    def gunpin(b):
        pst["gpin"].discard(b.name)

    def munpin(b):
        pst["mpin"].discard(b.name)
    ob_bufs = [P.view("ob_d%d" % i, ob_d[i]) for i in range(NT_OWN)]
    x1_bufs = [P.view("x1_d%d" % i, x1_d[i]) for i in range(NT_OWN)]
    out_bufs = [P.view("out_d%d" % i, out_d[i * 128:(i + 1) * 128, :]) for i in range(NT_OWN)]

    DIRM = [dict(Linc="UT", Rem="SL", NA="NSL", NT="NUT"),
            dict(Linc="LT", Rem="SU", NA="NSU", NT="NLT")]

    def V(b, ap=None):
        return (b, b.t if ap is None else ap)

    load_i = [0]

    def front_steps(ti, d, with_out, final, kk):
        par = kk % 2
        dm = DIRM[d]
        Linc, Rem = C[dm["Linc"]], C[dm["Rem"]]
        I_f, ones = C["ident"], C["ones"]
        gqT, gkT, mqT, mkT, mvx = gqT2[par], gkT2[par], mqT2[par], mkT2[par], mvx3[kk % 3]
        ktok, vtok, mkt, zs = ktok2[par], vtok2[par], mkt2[par], zs3[kk % 3]
        gpre, gcat, gsm, sm2 = gpre2[par], gcat2[par], gsm_2[par], sm2_2[par]
        tot_b = tot3[kk % 3]
        xb = xt[0]
        F = []
        fm_groups = [1, 2] if not with_out else [0, 1, 2]

        def f_norm():
            src = xT_d[:, ti * 128: ti * 128 + 132].rearrange("(j p) n -> p j n", p=128)
            O.dma((xb, xb.t[:, :, :]), src)
            sqv = pre.t[:, 0:8, :]
            O.act((pre, sqv), (xb, xb.t[:, :, :]), AF.Square)
        F.append(f_norm)

        def f_norm2():
            bk = falloc()
            for j in range(8):
                O.mm((bk, bk.t[:, 0:132]), ones, (pre, pre.t[:, j, :]), start=(j == 0), stop=(j == 7))
            O.act((rstd, rstd.t[:, :]), (bk, bk.t[:, 0:132]), AF.Ln, scale=1.0 / 1024, bias=EPS)
            O.act((rstd, rstd.t[:, :]), (rstd, rstd.t[:, :]), AF.Exp, scale=-0.5)
            O.tt((hT, hT.t), (xb, xb.t), (rstd, bc_mid(rstd.t, 8)), ALU.mult)
        F.append(f_norm2)
        chs = [g * 4 + c for g in fm_groups for c in range(4)]
        for c0 in range(0, len(chs), 3):
            def f_fm(c0=c0):
                grp = chs[c0:c0 + 3]
                bk = falloc()
                for cc, ch in enumerate(grp):
                    for j in range(8):
                        O.mm((bk, bk.t[:, cc * 132:(cc + 1) * 132]), (wfm, wfm.t[:, j, ch * 128:(ch + 1) * 128]),
                             (hT, hT.t[:, j, :]), start=(j == 0), stop=(j == 7))
                n = len(grp)
                O.cp((pre, pre.t[:, grp[0]:grp[0] + n, :]),
                     (bk, bk.t[:, 0:132 * n].rearrange("p (c n) -> p c n", c=n)), eng="act")
            F.append(f_fm)
        cst = {}
        for g in fm_groups:
            for k in range(5):
                def f_conv(g=g, k=k):
                    if k == 0:
                        cst[g] = falloc()
                    bk = cst[g]
                    w_bc = (convw, bc_last(convw.t[:, g * 4:(g + 1) * 4, k], 128))
                    src_k = (pre, pre.t[:, g * 4:(g + 1) * 4, k:k + 128])
                    ct = cbufs[(g * 5 + k) % 2]
                    O.tt(V(ct), src_k, w_bc, ALU.mult, eng="pool")
                    O.mm((bk, bk.t[:, :]), (identb, identb.t), (ct, ct.t.rearrange("p a b -> p (a b)")),
                         start=(k == 0), stop=(k == 4))
                    if k == 4:
                        acc = (cacc, cacc.t[:, g * 4:(g + 1) * 4, :])
                        O.act(acc, (bk, pv4(bk)), AF.Silu)
                F.append(f_conv)
        for g in [g for g in fm_groups if g < 2]:
            def f_l2(g=g):
                acc = (cacc, cacc.t[:, g * 4:(g + 1) * 4, :])
                O.act(V(nsq), acc, AF.Square)
            F.append(f_l2)

            def f_l2b(g=g):
                acc = (cacc, cacc.t[:, g * 4:(g + 1) * 4, :])
                bk = falloc()
                for h in range(4):
                    O.mm((bk, pv4(bk)[:, h, :]), ones, (nsq, nsq.t[:, h, :]))
                O.act(V(nrs), (bk, pv4(bk)), AF.Ln, bias=EPS)
                O.act(V(nrs), V(nrs), AF.Exp, scale=-0.5)
                if g == 0:
                    O.stt(V(gqT), acc, 128.0 ** -0.5, V(nrs), ALU.mult, ALU.mult)
                else:
                    O.tt(acc, acc, V(nrs), ALU.mult)
                    O.cp(V(gkT), acc, eng="pool")
            F.append(f_l2b)

        def f_tr():
            bk = falloc()
            for h in range(4):
                O.tr((bk, pv4(bk)[:, h, :]), (cacc, cacc.t[:, 4 + h, :]), I_f)
            O.cp(V(ktok), (bk, pv4(bk)), eng="act")
            bk = falloc()
            for h in range(4):
                O.tr((bk, pv4(bk)[:, h, :]), (cacc, cacc.t[:, 8 + h, :]), I_f)
            O.cp(V(vtok), (bk, pv4(bk)), eng="act")
        F.append(f_tr)

        def tm_proj0(c0, n):
            bk = falloc()
            for j in range(8):
                O.mm((bk, bk.t[:, 0:n]), (hT, hT.t[:, j, 2:130]), (wtm, wtm.t[:, j, c0:c0 + n]),
                     start=(j == 0), stop=(j == 7))
            return bk

        def f_gates():
            bk = tm_proj0(2048, 32)
            O.tt((gpre, gpre.t[:, :]), (bk, bk.t[:, 0:32]), (gpadd, gpadd.t[:, :]), ALU.add)
            a_ = (gpre, gpre.t[:, 4 * d:4 * d + 4])
            b_ = (gpre, gpre.t[:, 8 + 4 * d:12 + 4 * d])
            i_ = (gpre, gpre.t[:, 16 + 4 * d:20 + 4 * d])
            f_ = (gpre, gpre.t[:, 24 + 4 * d:28 + 4 * d])
            g_ = (gcat, gcat.t[:, 0:4])
            lf_ = (gcat, gcat.t[:, 4:8])
            t0 = (gsm, gsm.t[:, 0:4]); t1 = (gsm, gsm.t[:, 4:8])
            beta = (gsm, gsm.t[:, 8:12])
            O.act(t0, a_, AF.Exp)
            O.act(t0, t0, AF.Ln, bias=1.0)
            O.tt(g_, t0, (nEa, nEa.t[:, 4 * d:4 * d + 4]), ALU.mult)
            O.act(t1, f_, AF.Exp, scale=-1.0)
            O.act(t1, t1, AF.Ln, bias=1.0)
            O.ts(lf_, t1, -1.0, op0=ALU.mult)
            O.act(t1, b_, AF.Exp, scale=-1.0)
            O.act(t1, t1, AF.Ln, bias=1.0)
            O.act(beta, t1, AF.Exp, scale=-1.0)
        F.append(f_gates)

        def f_gates2():
            i_ = (gpre, gpre.t[:, 16 + 4 * d:20 + 4 * d])
            beta = (gsm, gsm.t[:, 8:12])
            bk = falloc()
            gcat_v = (gcat, gcat.t[:, :])
            O.mm((bk, bk.t[:, 0:8]), Linc, gcat_v)
            O.mm((bk, bk.t[:, 8:16]), Rem, gcat_v)
            O.mm((bk, bk.t[:, 16:24]), C["blk0"], gcat_v)
            O.mm((bk, bk.t[:, 24:32]), C["blk1"], gcat_v)
            egc = (sm2, sm2.t[:, 0:4]); kbgs = (sm2, sm2.t[:, 4:8]); kdcs = (sm2, sm2.t[:, 8:12])
            kws = (sm2, sm2.t[:, 12:16]); tot = (tot_b, tot_b.t[:, 0:16])
            O.act(egc, (bk, bk.t[:, 0:4]), AF.Exp)
            O.tt(kbgs, egc, beta, ALU.mult)
            O.act(kdcs, (bk, bk.t[:, 8:12]), AF.Exp)
            O.act(kws, (bk, bk.t[:, 12:16]), AF.Exp)
            O.act((sm2, sm2.t[:, 16:20]), i_, AF.Exp)
            O.act(tot, (bk, bk.t[:, 16:32]), AF.Exp)
        F.append(f_gates2)
        if with_out:
            def f_mq():
                bk = falloc()
                for h in range(4):
                    ch = 12 + h
                    for j in range(8):
                        O.mm((bk, pv4(bk)[:, h, :]), (wfm, wfm.t[:, j, ch * 128:(ch + 1) * 128]),
                             (hT, hT.t[:, j, 2:130]), start=(j == 0), stop=(j == 7))
                O.cp(V(mqT), (bk, pv4(bk)), eng="act")
            F.append(f_mq)

        def f_mv():
            bk = tm_proj0(1024, 512)
            O.cp((mvx, mvx.t[:, :, 0:128]), (bk, pv4(bk)), eng="act")
        F.append(f_mv)

        def f_mk():
            bk = tm_proj0(1536, 512)
            O.act(V(mkt), (bk, pv4(bk)), AF.Copy, scale=128.0 ** -0.5)
        F.append(f_mk)

        def f_mk2():
            if with_out:
                bk2 = falloc()
                for h in range(4):
                    O.tr((bk2, pv4(bk2)[:, h, :]), (mkt, mkt.t[:, h, :]), I_f)
                O.cp(V(mkT), (bk2, pv4(bk2)), eng="act")
        if with_out:
            F.append(f_mk2)
        if final:
            def f_z():
                bk_z = tm_proj0(0, 512)
                O.act((zs, zs.t[:, 0:4, :]), (bk_z, pv4(bk_z)), AF.Silu)
            def f_o():
                bk_o = tm_proj0(512, 512)
                O.act((zs, zs.t[:, 4:8, :]), (bk_o, pv4(bk_o)), AF.Sigmoid)
            F.append(f_z); F.append(f_o)
        return F

    def back_steps(ti, d, with_out, final, kk, first):
        par = kk % 2
        dm = DIRM[d]
        Linc, Rem, NA, NTm = C[dm["Linc"]], C[dm["Rem"]], C[dm["NA"]], C[dm["NT"]]
        I_f, ones = C["ident"], C["ones"]
        gqT, gkT, mqT, mkT, mvx = gqT2[par], gkT2[par], mqT2[par], mkT2[par], mvx3[kk % 3]
        ktok, vtok, mkt, zs = ktok2[par], vtok2[par], mkt2[par], zs3[kk % 3]
        gpre, gcat, gsm, sm2 = gpre2[par], gcat2[par], gsm_2[par], sm2_2[par]
        tot_b = tot3[kk % 3]
        kdec, u_sb, wT, qkT, qdT = kdec_2[par], u_sb_2[par], wT_2[par], qkT_2[par], qdT_2[par]
        kw, pT, mqd = kw_2[par], pT_2[par], mqd_2[par]
        SLf, UTf, Dmf = SLg, UTg, Dm
        idb = (identb, identb.t)
        NAb = (negb, negb.t[:, 0, :]); NTb = (negb, negb.t[:, 1, :])
        beta_bc = (gsm, bc_last(gsm.t[:, 8:12], 128))
        blks = (0, 1) if d == 0 else (1, 0)
        G = []

        def g1():
            if negb_dir[0] != d:
                negb_dir[0] = d
                O.cp(NAb, NA, eng="pool")
                O.cp(NTb, NTm, eng="pool")
            O.tt(V(SLg), (Rem[0], bc_mid(Rem[1], 4)), (gcat, bc_last(gcat.t[:, 0:4], 128)), ALU.mult, eng="pool")
            bkK = galloc(); bkD = galloc()
            for h in range(4):
                O.mm((bkK, pv4(bkK)[:, h, :]), (gkT, gkT.t[:, h, :]), (gkT, gkT.t[:, h, :]))
            for h in range(4):
                O.mm((bkD, pv4(bkD)[:, h, :]), Linc, (SLg, SLg.t[:, h, :]), start=True, stop=False)
                O.mm((bkD, pv4(bkD)[:, h, :]), idb, NAb, start=False, stop=True)
            O.act(V(Dm), (bkD, pv4(bkD)), AF.Exp)
            O.tt(V(A0), (bkK, pv4(bkK)), V(Dm), ALU.mult)
            O.tt(V(A0), V(A0), beta_bc, ALU.mult)
        G.append(g1)

        def g_sc():
            O.tt(V(vb), V(vtok), beta_bc, ALU.mult, eng="pool")
            O.tt(V(kbg), V(ktok), (sm2, bc_last(sm2.t[:, 4:8], 128)), ALU.mult, eng="pool")
            O.tt(V(kdec), V(ktok), (sm2, bc_last(sm2.t[:, 8:12], 128)), ALU.mult, eng="pool")

        def g2():
            bk = galloc()
            for h in range(4):
                O.tr((bk, pv4(bk)[:, h, :]), (A0, A0.t[:, h, :]), I_f)
            O.cp(V(A0T), (bk, pv4(bk)), eng="act")
            O.tt(V(Pm), (I_f[0], bc_mid(I_f[1], 4)), V(A0T), ALU.subtract)
        G.append(g2)
        chain = [(A0, A0T)]
        for lvl in range(5):
            chain.append((Mb0, MTb0) if lvl % 2 == 0 else (A0, A0T))
        def emit_sq(lvl):
            M, MT = chain[lvl]; M2, M2T = chain[lvl + 1]
            bk = galloc()
            for h in range(4):
                O.mm((bk, pv4(bk)[:, h, :]), (MT, MT.t[:, h, :]), (M, M.t[:, h, :]))
            bk2 = None
            if lvl < 4:
                bk2 = galloc()
                for h in range(4):
                    O.mm((bk2, pv4(bk2)[:, h, :]), (M, M.t[:, h, :]), (MT, MT.t[:, h, :]))
            return bk, bk2

        def evac_sq(lvl, bk, bk2):
            M2, M2T = chain[lvl + 1]
            O.cp(V(M2), (bk, pv4(bk)), eng="act")
            if bk2 is not None:
                O.cp(V(M2T), (bk2, pv4(bk2)), eng="act")

        def emit_pr(lvl):
            M2, M2T = chain[lvl + 1]
            bk3 = galloc()
            for h in range(4):
                O.mm((bk3, pv4(bk3)[:, h, :]), (M2, M2.t[:, h, :]), (Pm, Pm.t[:, h, :]))
            O.tt(V(Pm), V(Pm), (bk3, pv4(bk3)), ALU.add)

        def g_n0():
            bk, bk2 = emit_sq(0)
            evac_sq(0, bk, bk2)
        G.append(g_n0)
        for lvl in range(1, 5):
            def g_n(lvl=lvl):
                emit_pr(lvl - 1)
                bk, bk2 = emit_sq(lvl)
                evac_sq(lvl, bk, bk2)
            G.append(g_n)

        def g_n5():
            emit_pr(4)
        G.append(g_n5)

        def g3():
            O.cp(V(TTb), V(Pm), eng="act")
            bkU = galloc(); bkW = galloc()
            for h in range(4):
                O.mm((bkU, pv4(bkU)[:, h, :]), (TTb, TTb.t[:, h, :]), (vb, vb.t[:, h, :]))
            for h in range(4):
                O.mm((bkW, pv4(bkW)[:, h, :]), (kbg, kbg.t[:, h, :]), (TTb, TTb.t[:, h, :]))
            O.cp(V(u_sb), (bkU, pv4(bkU)), eng="act")
            O.cp(V(wT), (bkW, pv4(bkW)), eng="act")
        if with_out:
            def g_q1():
                O.tt(V(UTg), (Linc[0], bc_mid(Linc[1], 4)), (gcat, bc_last(gcat.t[:, 0:4], 128)), ALU.mult, eng="pool")
                bkQ = galloc(); bkD = galloc()
                for h in range(4):
                    O.mm((bkQ, pv4(bkQ)[:, h, :]), (gkT, gkT.t[:, h, :]), (gqT, gqT.t[:, h, :]))
                for h in range(4):
                    O.mm((bkD, pv4(bkD)[:, h, :]), (SLg, SLg.t[:, h, :]), Linc, start=True, stop=False)
                    O.mm((bkD, pv4(bkD)[:, h, :]), idb, NTb, start=False, stop=True)
                O.act(V(Dm), (bkD, pv4(bkD)), AF.Exp)
                O.tt(V(qkT), (bkQ, pv4(bkQ)), V(Dm), ALU.mult)

            def g_q2():
                bkG = galloc()
                for h in range(4):
                    O.mm((bkG, pv4(bkG)[:, h, :]), ones, (UTg, UTg.t[:, h, :]))
                O.act(V(Dm), (bkG, pv4(bkG)), AF.Exp)
                O.tt(V(qdT), V(gqT), V(Dm), ALU.mult)
            G.insert(3, g_q1)
            G.insert(5, g_q2)
        G.insert(2, g_sc)
        G.append(g3)
        S_, Sb_ = Sg, Sgb
        gst = {}
        for bi, blk in enumerate(blks):
            def g_blk(bi=bi, blk=blk):
                r0, r1 = blk * 64, blk * 64 + 64
                bkS = malloc_()
                for h in range(4):
                    O.mm((bkS, pv4(bkS)[r0:r1, h, :]), (wT, wT.t[:, h, r0:r1]), (Sb_, Sb_.t[:, h, :]))
                O.tt((vnew, vnew.t[r0:r1, :, :]), (u_sb, u_sb.t[r0:r1, :, :]), (bkS, pv4(bkS)[r0:r1, :, :]), ALU.subtract)
                if with_out:
                    bkO = malloc_()
                    for h in range(4):
                        O.mm((bkO, pv4(bkO)[r0:r1, h, :]), (qdT, qdT.t[:, h, r0:r1]), (Sb_, Sb_.t[:, h, :]), start=True, stop=False)
                        O.mm((bkO, pv4(bkO)[r0:r1, h, :]), (qkT, qkT.t[r0:r1, h, r0:r1]), (vnew, vnew.t[r0:r1, h, :]), start=False, stop=True)
                    O.cp((o_sb, o_sb.t[r0:r1, 0:4, :]), (bkO, pv4(bkO)[r0:r1, :, :]), eng="act")
                bkdS = malloc_()
                for h in range(4):
                    O.mm((bkdS, pv4(bkdS)[:, h, :]), (kdec, kdec.t[r0:r1, h, :]), (vnew, vnew.t[r0:r1, h, :]))
                O.tt(V(S_), V(S_), (tot_b, bc_last(tot_b.t[:, 8 * blk:8 * blk + 4], 128)), ALU.mult)
                O.tt(V(S_), V(S_), (bkdS, pv4(bkdS)), ALU.add)
                O.cp(V(Sb_), V(S_), eng="act")
            G.append(g_blk)

        Mx = []

        def m_kw():
            O.tt(V(kw), V(mkt), (sm2, bc_last(sm2.t[:, 12:16], 128)), ALU.mult, eng="pool")
            O.tt((mvx, mvx.t[:, :, 0:128]), (mvx, mvx.t[:, :, 0:128]), (sm2, bc_last(sm2.t[:, 16:20], 128)), ALU.mult, eng="pool")
            O.cp((mvx, mvx.t[:, :, 128]), (sm2, sm2.t[:, 16:20]), eng="pool")
        Mx.append(m_kw)
        if with_out:
            def m_p():
                O.tt(V(SLf), (Rem[0], bc_mid(Rem[1], 4)), (gcat, bc_last(gcat.t[:, 4:8], 128)), ALU.mult, eng="pool")
                bkQ = galloc(); bkD = galloc()
                for h in range(4):
                    O.mm((bkQ, pv4(bkQ)[:, h, :]), (mkT, mkT.t[:, h, :]), (mqT, mqT.t[:, h, :]))
                for h in range(4):
                    O.mm((bkD, pv4(bkD)[:, h, :]), (SLf, SLf.t[:, h, :]), Linc, start=True, stop=False)
                    O.mm((bkD, pv4(bkD)[:, h, :]), idb, NTb, start=False, stop=True)
                O.act(V(Dmf), (bkD, pv4(bkD)), AF.Exp)
                O.tt(V(pT), (bkQ, pv4(bkQ)), V(Dmf), ALU.mult)
            Mx.append(m_p)

            def m_qd():
                O.tt(V(UTf), (Linc[0], bc_mid(Linc[1], 4)), (gcat, bc_last(gcat.t[:, 4:8], 128)), ALU.mult, eng="pool")
                bkG = galloc()
                for h in range(4):
                    O.mm((bkG, pv4(bkG)[:, h, :]), ones, (UTf, UTf.t[:, h, :]))
                O.act(V(Dmf), (bkG, pv4(bkG)), AF.Exp)
                O.tt(V(mqd), V(mqT), V(Dmf), ALU.mult)
            Mx.append(m_qd)
        C_, Cb_ = Cm, Cmb
        mst = {}
        for bi, blk in enumerate(blks):
            def m_blk(bi=bi, blk=blk):
                r0, r1 = blk * 64, blk * 64 + 64
                if with_out:
                    bkN = [malloc_(), malloc_()]
                    for h in range(4):
                        o_ap = bkN[h // 2].t[r0:r1, (h % 2) * 130:(h % 2) * 130 + 129]
                        O.mm((bkN[h // 2], o_ap), (mqd, mqd.t[:, h, r0:r1]), (Cb_, Cb_.t[:, h, 0:129]), start=True, stop=False)
                        O.mm((bkN[h // 2], o_ap), (pT, pT.t[r0:r1, h, r0:r1]), (mvx, mvx.t[r0:r1, h, 0:129]), start=False, stop=True)
                    dmax = (gsm2, gsm2.t[r0:r1, 0:4])
                    for q in range(2):
                        nv = bkN[q].t[r0:r1, 0:260].rearrange("p (h s) -> p h s", h=2)
                        dq = (gsm2, gsm2.t[r0:r1, 2 * q:2 * q + 2])
                        O.act(dq, (bkN[q], nv[:, :, 128]), AF.Abs)
                        O.ts(dq, dq, 1.0, op0=ALU.max)
                    O.recip(dmax, dmax)
                    for q in range(2):
                        nv = bkN[q].t[r0:r1, 0:260].rearrange("p (h s) -> p h s", h=2)
                        O.tt((o_sb, o_sb.t[r0:r1, 4 + 2 * q:6 + 2 * q, :]), (bkN[q], nv[:, :, 0:128]),
                             (gsm2, bc_last(gsm2.t[r0:r1, 2 * q:2 * q + 2], 128)), ALU.mult)
                bkC = [malloc_(), malloc_()]
                for h in range(4):
                    O.mm((bkC[h // 2], bkC[h // 2].t[:, (h % 2) * 130:(h % 2) * 130 + 129]), (kw, kw.t[r0:r1, h, :]),
                         (mvx, mvx.t[r0:r1, h, 0:129]))
                O.tt(V(C_), V(C_), (tot_b, bc_last(tot_b.t[:, 4 + 8 * blk:8 + 8 * blk], 130)), ALU.mult)
                for q in range(2):
                    O.tt((C_, C_.t[:, 2 * q:2 * q + 2, 0:129]), (C_, C_.t[:, 2 * q:2 * q + 2, 0:129]),
                         (bkC[q], bkC[q].t[:, 0:260].rearrange("p (h s) -> p h s", h=2)[:, :, 0:129]), ALU.add)
                O.cp((Cb_, Cb_.t[:, :, 0:129]), (C_, C_.t[:, :, 0:129]), eng="act")
            Mx.append(m_blk)
        g_pre = [s_ for s_ in G if s_.__name__ != "g_blk"]
        g_rec = [s_ for s_ in G if s_.__name__ == "g_blk"]
        m_pre = [s_ for s_ in Mx if s_.__name__ != "m_blk"]
        m_rec = [s_ for s_ in Mx if s_.__name__ == "m_blk"]
        Pl = list(g_pre)
        pos = 8 if with_out else 4
        for s_ in m_pre:
            Pl.insert(min(pos, len(Pl) - 1), s_)
            pos += 2
        B = []
        if first:
            B.append(reset_states)
        for s1, s2 in zip(g_rec, m_rec):
            B.append(s1); B.append(s2)
        own_i = ti - NT_OTHER
        if with_out and not final:
            def o_store():
                O.dma((ob_bufs[own_i], ob_d[own_i].rearrange("p (h d) -> p h d", h=8)), (o_sb, o_sb.t[:, :, :]))
            B.append(o_store)
        if final:
            def o_fin1():
                O.dma((ob_sb, ob_sb.t[:, :, :]), (ob_bufs[own_i], ob_d[own_i].rearrange("p (h d) -> p h d", h=8)))
                O.dma((xtok, xtok.t[:, :]), xown_d[own_i * 128:(own_i + 1) * 128, :])
                O.tt(V(o_sb), V(o_sb), V(ob_sb), ALU.add)
                O.tt(V(ob_sb), V(o_sb), V(o_sb), ALU.mult)
                ssq = (gsm2, gsm2.t[:, 8:16])
                O.red(ssq, V(ob_sb), ALU.add)
                O.act(ssq, ssq, AF.Ln, scale=1.0 / 128, bias=EPS)
                O.act(ssq, ssq, AF.Exp, scale=-0.5)
                O.tt(V(o_sb), V(o_sb), (gsm2, bc_last(gsm2.t[:, 8:16], 128)), ALU.mult)
                O.tt((o_sb, o_sb.t[:, 0:4, :]), (o_sb, o_sb.t[:, 0:4, :]), V(gng), ALU.mult)
                O.tt((o_sb, o_sb.t[:, 4:8, :]), (o_sb, o_sb.t[:, 4:8, :]), V(mng), ALU.mult)
                O.tt(V(y_sb), V(o_sb), V(zs), ALU.mult)

            def o_fin2():
                bk = malloc_()
                bkb = bk.t.bitcast(BF16)
                for c8 in range(8):
                    O.tr((bk, bkb[:, c8 * 128:(c8 + 1) * 128]), (y_sb, y_sb.t[:, c8, :]), (identb, identb.t[:, :]))
                O.cp(V(yT), (bk, bkb[:, :].rearrange("p (c t) -> p c t", c=8)), eng="act")

            def o_fin3():
                for half in range(2):
                    bk = malloc_()
                    for j in range(8):
                        O.mm((bk, bk.t[:, :]), (yT, yT.t[:, j, :]), (wout, wout.t[:, j, half * 512:(half + 1) * 512]),
                             start=(j == 0), stop=(j == 7))
                    O.tt((x1, x1.t[:, half * 512:(half + 1) * 512]), (xtok, xtok.t[:, half * 512:(half + 1) * 512]),
                         (bk, bk.t[:, :]), ALU.add)
                O.dma((x1_bufs[own_i], x1_d[own_i]), (x1, x1.t[:, :]))
            B += [o_fin1, o_fin2, o_fin3]
        return Pl, B

    sched = [(ti, 1, True, False) for ti in range(NT - 1, NT_OTHER - 1, -1)]
    sched += [(ti, 0, False, False) for ti in range(0, NT_OTHER)]
    sched += [(ti, 0, True, True) for ti in range(NT_OTHER, NT)]
    conv_i = 0
    nS = len(sched)
    pending = {}
    for j in range(-2, nS):
        if phase2 and j >= 1:
            left_tiles = nS - j
            todo = -(-(n_conv - conv_i) // max(1, left_tiles - 2)) if left_tiles > 2 else n_conv - conv_i
            for _ in range(min(todo, n_conv - conv_i)):
                conv_chunk(conv_i); conv_i += 1
        lists = []
        if 0 <= j < nS:
            lists.append(pending.pop(j))
        if 0 <= j + 1 < nS:
            Pl, Bl = back_steps(*sched[j + 1], j + 1, (j + 1) == 0 or (j + 1) == NT_OWN)
            pending[j + 1] = Bl
            lists.append(Pl)
        if 0 <= j + 2 < nS:
            lists.append(front_steps(*sched[j + 2], j + 2))
        idx = [0] * len(lists)
        tot_n = max([len(l) for l in lists] + [1])
        for step in range(tot_n):
            for li, l in enumerate(lists):
                want = ((step + 1) * len(l) + tot_n - 1) // tot_n
                while idx[li] < min(want, len(l)):
                    l[idx[li]](); idx[li] += 1
    assert (not phase2) or conv_i == n_conv
    if not phase2:
        for i in range(NT_OWN):
            O.dma((xtok, xtok.t[:, :]), (x1_bufs[i], x1_d[i]))
            O.dma((out_bufs[i], out_d[i * 128:(i + 1) * 128, :]), (xtok, xtok.t[:, :]))
    else:
        P.barrier()
        P.release(PH_MARK)
        phase2_build(nc, P, O, C, identb, iot, banks, NT_OWN, x1_bufs, x1_d, out_bufs, out_d,
                     wq_d, keysT_d, n2g_d, nfg_d, UTs, Vs, UTs_d, Vs_d)
    P.wait_all("sp", out_bufs)
    P.emit()
    return nc


def phase2_build(nc, P, O, C, identb, iot, banks, NT_OWN, x1_bufs, x1_d, out_bufs, out_d,
                 wq_d, keysT_d, n2g_d, nfg_d, UTs, Vs, UTs_d, Vs_d):
    I_f = C["ident"]
    TB = 256
    NB = NT_OWN // 2
    IG = 2
    TS = 8
    obank = [[banks[0], banks[1]], [banks[2], banks[3]]]
    pools = {"loop": [banks[4], banks[5], banks[6]], "prep": [banks[7]]}
    pidx = {"loop": 0, "prep": 0}

    def pbank(who):
        b = pools[who][pidx[who] % len(pools[who])]
        pidx[who] += 1
        return b

    def pv4(b):
        return b.t[:, :].rearrange("p (h s) -> p h s", h=4)

    def V(b, ap=None):
        return (b, b.t if ap is None else ap)

    wq = P.sb("wq", [128, 8, 2048], BF16)
    for j in range(8):
        O.dma((wq, wq.t[:, j, :]), wq_d[j * 128:(j + 1) * 128, :], eng="pool")
    keysT = P.sb("keysT", [128, 16, 128], F32)
    O.dma((keysT, keysT.t.rearrange("p a b -> p (a b)")), keysT_d)
    n2g = P.sb("n2g", [128, 1024], F32)
    O.dma(V(n2g), n2g_d)
    nfg = P.sb("nfg", [128, 1024], F32)
    O.dma(V(nfg), nfg_d)
    x1p = [P.sb("x1p0", [128, 1024], F32)] * 2
    xr = [P.sb("xr0", [128, 1024], F32)] * 2
    h2 = P.sb("h2", [128, 8, 128], BF16)
    h2T = [P.sb("h2T%d" % i, [128, 8, TB], BF16) for i in range(2)]
    qT = P.sb("qT", [128, 16, 128], F32)
    cand = (qT, qT.t.rearrange("p a b -> p (a b)").rearrange("p (h j r) -> p h j r", h=8, j=16))
    sc = P.sb("sc", [128, 16, 128], F32)
    oh = (sc, sc.t.rearrange("p a b -> p (a b)").rearrange("p (h j r) -> p h j r", h=8, j=16))
    top = P.sb("top", [128, 16, 16], F32)
    ti = P.sb("ti", [128, 16, 16], U32)
    tif = P.sb("tif", [128, 16, 16], F32)
    scr = P.sb("scr", [128, 256], F32)
    best = P.sb("best", [128, 8, 16], F32)
    pos = P.sb("pos", [128, 8, 16], U32)
    rr = P.sb("rr", [128, 2, 8, 16], U32)
    rrf = P.sb("rrf", [128, 2, 8, 16], F32)
    abw = P.sb("abw", [128, 3, 128], F32)
    abwT = [P.sb("abwT%d" % i, [128, 3, TB], F32) for i in range(2)]
    abT16 = [P.sb("abT16_%d" % i, [128, 2, TB], BF16) for i in range(2)]
    iot16 = P.sb("iot16", [128, 128], BF16)
    O.cp((iot16, iot16.t), (iot, iot.t[:, 16:144]), eng="dve")
    sm = P.sb("p2sm", [128, 64], F32)
    smf = P.sb("p2smf", [128, 8], F32)
    Acol = [P.sb("Acol%d" % i, [128, TS, 128], BF16) for i in range(2)]
    Bcol = [P.sb("Bcol%d" % i, [128, TS, 128], BF16) for i in range(2)]
    Gs = P.sb("Gs", [128, TB, 128], BF16)
    UTb = [P.sb("UTb%d" % i, [128, IG, 8, 128], BF16) for i in range(3)]
    Vb = [P.sb("Vb%d" % i, [128, IG, 1024], BF16) for i in range(3)]
    actb = [P.sb("actb%d" % i, [128, TB], BF16) for i in range(3)]
    GA = [P.sb("GA%d" % i, [128, TB], BF16) for i in range(3)]
    topv = top.t.rearrange("p (h q) r -> p h q r", q=2)
    tifv = tif.t.rearrange("p (h q) r -> p h q r", q=2)
    candh = (qT, cand[1][:, 0, 0:8, :])
    sqscr = (sc, sc.t.rearrange("p a b -> p (a b)")[:, 0:1024])
    sqscr2 = None

    def prep_steps(blk, pb):
        st = []
        hT_ = h2T[pb]; aT_ = abwT[pb]; a16_ = abT16[pb]
        for tt in range(2):
            tix = blk * 2 + tt
            xb = x1p[tt]
            tsl = slice(tt * 128, (tt + 1) * 128)

            def s_norm(tix=tix, xb=xb, tt=tt):
                O.dma(V(xb), (x1_bufs[tix], x1_d[tix]))
                O.act(sqscr, V(xb), AF.Square, accum=(sm, sm.t[:, 0:1]))
                O.act((sm, sm.t[:, 0:1]), (sm, sm.t[:, 0:1]), AF.Sqrt, scale=1.0 / 1024, bias=EPS)
                O.recip((sm, sm.t[:, 0:1]), (sm, sm.t[:, 0:1]))
                O.stt((h2, h2.t.rearrange("p a b -> p (a b)")), V(xb), (sm, sm.t[:, 0:1]), V(n2g), ALU.mult, ALU.mult)
            st.append(s_norm)

            def s_normT(tt=tt):
                bk = pbank("prep")
                bkb = bk.t.bitcast(BF16)
                for c8 in range(8):
                    O.tr((bk, bkb[:, c8 * 128:(c8 + 1) * 128]), (h2, h2.t[:, c8, :]), V(identb))
                O.cp((hT_, hT_.t[:, :, tt * 128:(tt + 1) * 128]), (bk, bkb[:, :].rearrange("p (c t) -> p c t", c=8)), eng="act")
            st.append(s_normT)
            for g4 in range(4):
                def s_q(g4=g4, tsl=tsl):
                    bk = pbank("prep")
                    for q in range(4):
                        hp = g4 * 4 + q
                        for j in range(8):
                            O.mm((bk, pv4(bk)[:, q, :]), (wq, wq.t[:, j, hp * 128:(hp + 1) * 128]), (hT_, hT_.t[:, j, tsl]),
                                 start=(j == 0), stop=(j == 7))
                    O.cp((qT, qT.t[:, g4 * 4:(g4 + 1) * 4, :]), (bk, pv4(bk)), eng="act")
                st.append(s_q)
            for g4 in range(4):
                def s_sc(g4=g4):
                    bk = pbank("prep")
                    for q in range(4):
                        hp = g4 * 4 + q
                        O.mm((bk, pv4(bk)[:, q, :]), (qT, qT.t[:, hp, :]), (keysT, keysT.t[:, hp, :]))
                    O.cp((sc, sc.t[:, g4 * 4:(g4 + 1) * 4, :]), (bk, pv4(bk)), eng="act")
                st.append(s_sc)
            for hp in range(16):
                def s_top(hp=hp):
                    s_hp = sc.t[:, hp, :]
                    t8a = top.t[:, hp, 0:8]; t8b = top.t[:, hp, 8:16]
                    i8a = ti.t[:, hp, 0:8]; i8b = ti.t[:, hp, 8:16]
                    s128 = scr.t[:, 0:128]
                    P.op("dve", lambda e: e.max(out=t8a, in_=s_hp), reads=[sc], writes=[top])
                    P.op("dve", lambda e: e.max_index(out=i8a, in_max=t8a, in_values=s_hp), reads=[sc, top], writes=[ti])
                    P.op("dve", lambda e: e.match_replace(out=s128, in_to_replace=t8a, in_values=s_hp, imm_value=-1e30), reads=[sc, top], writes=[scr])
                    P.op("dve", lambda e: e.max(out=t8b, in_=s128), reads=[scr], writes=[top])
                    P.op("dve", lambda e: e.max_index(out=i8b, in_max=t8b, in_values=s128), reads=[scr, top], writes=[ti])
                st.append(s_top)

            def s_cand():
                O.cp(V(tif), V(ti), eng="dve")
                O.tt(cand, (top, topv[:, :, 0, :].unsqueeze(3).to_broadcast([128, 8, 16, 16])),
                     (top, topv[:, :, 1, :].unsqueeze(2).to_broadcast([128, 8, 16, 16])), ALU.add)
            st.append(s_cand)
            for h in range(8):
                def s_best(h=h):
                    c_h = cand[1][:, h, :, :].rearrange("p a b -> p (a b)")
                    b8a = best.t[:, h, 0:8]; b8b = best.t[:, h, 8:16]
                    p8a = pos.t[:, h, 0:8]; p8b = pos.t[:, h, 8:16]
                    s256 = scr.t[:, :]
                    P.op("dve", lambda e: e.max(out=b8a, in_=c_h), reads=[qT], writes=[best])
                    P.op("dve", lambda e: e.max_index(out=p8a, in_max=b8a, in_values=c_h), reads=[qT, best], writes=[pos])
                    P.op("dve", lambda e: e.match_replace(out=s256, in_to_replace=b8a, in_values=c_h, imm_value=-1e30), reads=[qT, best], writes=[scr])
                    P.op("dve", lambda e: e.max(out=b8b, in_=s256), reads=[scr], writes=[best])
                    P.op("dve", lambda e: e.max_index(out=p8b, in_max=b8b, in_values=s256), reads=[scr, best], writes=[pos])
                st.append(s_best)

            def s_idx():
                P.op("dve", lambda e: e.tensor_single_scalar(out=rr.t[:, 0, :, :], in_=pos.t, scalar=4, op=ALU.logical_shift_right), reads=[pos], writes=[rr])
                P.op("dve", lambda e: e.tensor_single_scalar(out=rr.t[:, 1, :, :], in_=pos.t, scalar=15, op=ALU.bitwise_and), reads=[pos], writes=[rr])
                O.cp(V(rrf), V(rr), eng="dve")
            st.append(s_idx)
            for q in range(2):
                def s_lk(q=q):
                    O.tt(oh, (iot, iot.t[:, 0:16].unsqueeze(1).unsqueeze(1).to_broadcast([128, 8, 16, 16])),
                         (rrf, rrf.t[:, q, :, :].unsqueeze(3).to_broadcast([128, 8, 16, 16])), ALU.is_equal)
                    O.tt(oh, oh, (tif, tifv[:, :, q, :].unsqueeze(2).to_broadcast([128, 8, 16, 16])), ALU.mult)
                    O.red((abw, abw.t[:, q, :].rearrange("p (h j) -> p h j", h=8)), oh, ALU.add)
                st.append(s_lk)

            def s_gate(tsl=tsl):
                O.tt(candh, V(best), (best, bc_last(best.t[:, :, 0], 16)), ALU.subtract)
                O.act(candh, candh, AF.Exp)
                O.red((sm, sm.t[:, 8:16]), candh, ALU.add)
                O.recip((sm, sm.t[:, 8:16]), (sm, sm.t[:, 8:16]))
                O.tt((abw, abw.t[:, 2, :].rearrange("p (h j) -> p h j", h=8)), candh,
                     (sm, bc_last(sm.t[:, 8:16], 16)), ALU.mult)
            st.append(s_gate)

            def s_gateT(tsl=tsl):
                bk = pbank("prep")
                for q in range(3):
                    O.tr((bk, bk.t[:, q * 128:(q + 1) * 128]), (abw, abw.t[:, q, :]), I_f)
                O.cp((aT_, aT_.t[:, :, tsl]), (bk, bk.t[:, 0:384].rearrange("p (q t) -> p q t", q=3)), eng="act")
                O.cp((a16_, a16_.t[:, :, tsl]), (bk, bk.t[:, 0:256].rearrange("p (q t) -> p q t", q=2)), eng="act")
            st.append(s_gateT)
        return st

    def scatter(pb):
        aT_ = abwT[pb]; a16_ = abT16[pb]
        for sb_i in range(TB // TS):
            t0 = sb_i * TS
            Ac = Acol[sb_i % 2]; Bc = Bcol[sb_i % 2]
            io_bc = (iot16, iot16.t.unsqueeze(1).to_broadcast([128, TS, 128]))
            O.tt(V(Ac), io_bc, (a16_, bc_last(a16_.t[:, 0, t0:t0 + TS], 128)), ALU.is_equal)
            O.tt(V(Ac), V(Ac), (aT_, bc_last(aT_.t[:, 2, t0:t0 + TS], 128)), ALU.mult, eng="pool")
            O.tt(V(Bc), io_bc, (a16_, bc_last(a16_.t[:, 1, t0:t0 + TS], 128)), ALU.is_equal)
            for q4 in range(TS // 4):
                bk = pbank("loop" if (sb_i * (TS // 4) + q4) % 4 else "prep")
                for q in range(4):
                    tl = q4 * 4 + q
                    O.mm((bk, pv4(bk)[:, q, :]), (Ac, Ac.t[:, tl, :]), (Bc, Bc.t[:, tl, :]))
                O.cp((Gs, Gs.t[:, t0 + q4 * 4:t0 + q4 * 4 + 4, :]), (bk, pv4(bk)), eng="act")

    NG = 128 // IG

    def load_group(g):
        ub = UTb[g % 3]; vb_ = Vb[g % 3]
        O.dma((ub, ub.t.rearrange("p a b c -> p (a b c)")), (UTs, UTs_d[:, g * IG * 1024:(g + 1) * IG * 1024]), eng="sp")
        O.dma((vb_, vb_.t.rearrange("p a b -> p (a b)")), (Vs, Vs_d[:, g * IG * 1024:(g + 1) * IG * 1024]), eng="sp")

    def emit_v(ga, vb_, il, i2):
        for tt in range(2):
            for dh in range(2):
                O.mm((obank[tt][dh], obank[tt][dh].t[:, :]), (ga, ga.t[:, tt * 128:(tt + 1) * 128]),
                     (vb_, vb_.t[:, il, dh * 512:(dh + 1) * 512]), start=(i2 == 0), stop=(i2 == 127))

    def expert_loop(pb, side):
        hT_ = h2T[pb]
        per = (len(side) + 127) // 128 if side else 0
        si = 0
        load_group(0)
        load_group(1)
        pendq = []
        for g in range(NG):
            ub = UTb[g % 3]; vb_ = Vb[g % 3]
            for il in range(IG):
                i2 = g * IG + il
                bk = pbank("loop")
                for j in range(8):
                    O.mm((bk, bk.t[:, 0:TB]), (ub, ub.t[:, il, j, :]), (hT_, hT_.t[:, j, :]), start=(j == 0), stop=(j == 7))
                ab = actb[i2 % 3]; ga = GA[i2 % 3]
                O.act(V(ab), (bk, bk.t[:, 0:TB]), AF.Gelu)
                O.tt(V(ga), V(ab), (Gs, Gs.t[:, :, i2]), ALU.mult, eng="pool")
                pendq.append((ga, vb_, il, i2))
                if len(pendq) > 2:
                    emit_v(*pendq.pop(0))
                if il == 1 and g + 2 < NG:
                    load_group(g + 2)
                for _ in range(per):
                    if si < len(side):
                        side[si](); si += 1
        while pendq:
            emit_v(*pendq.pop(0))
        while si < len(side):
            side[si](); si += 1

    def final(blk):
        for tt in range(2):
            tix = blk * 2 + tt
            xb = xr[tt]
            O.dma(V(xb), (x1_bufs[tix], x1_d[tix]))
            for dh in range(2):
                O.tt((xb, xb.t[:, dh * 512:(dh + 1) * 512]), (xb, xb.t[:, dh * 512:(dh + 1) * 512]),
                     (obank[tt][dh], obank[tt][dh].t[:, :]), ALU.add)
            O.act((Gs, Gs.t[:, 0:8, :].rearrange("p a b -> p (a b)")), V(xb), AF.Square, accum=(smf, smf.t[:, tt:tt + 1]))
            O.act((smf, smf.t[:, tt:tt + 1]), (smf, smf.t[:, tt:tt + 1]), AF.Sqrt, scale=1.0 / 1024, bias=EPS)
            O.recip((smf, smf.t[:, tt:tt + 1]), (smf, smf.t[:, tt:tt + 1]))
            O.stt(V(xb), V(xb), (smf, smf.t[:, tt:tt + 1]), V(nfg), ALU.mult, ALU.mult)
            O.dma((out_bufs[tix], out_d[tix * 128:(tix + 1) * 128, :]), V(xb))

    for s_ in prep_steps(0, 0):
        s_()
    for blk in range(NB):
        pb = blk % 2
        scatter(pb)
        side = prep_steps(blk + 1, 1 - pb) if blk + 1 < NB else []
        expert_loop(pb, side)
        final(blk)


def prep_shared(inp):
    f32 = np.float32

    def rep(v):
        return np.repeat(np.asarray(v, f32).reshape(1, -1), 128, axis=0)
    d = {}
    keys = np.asarray(inp["peer_keys"], f32)[0]
    d["keysT"] = keys.transpose(3, 0, 1, 2).reshape(128, 2048)
    d["wq"] = np.asarray(inp["peer_wq"], f32)[0]
    d["n2g"] = rep(np.asarray(inp["norm2_g"], f32)[0])
    d["nfg"] = rep(np.asarray(inp["normf_g"], f32))
    U = np.asarray(inp["peer_u"], f32)[0]
    d["UT"] = U.reshape(128, 128, 8, 128).transpose(3, 1, 2, 0).reshape(128, 128 * 1024)
    d["Vt"] = np.asarray(inp["peer_v"], f32)[0].reshape(128, 128 * 1024)
    d["iotas"] = np.concatenate([rep(np.arange(16)), rep(np.arange(128))], axis=1)
    d["w_out"] = np.asarray(inp["w_out"], f32)[0]
    d["consts"] = CONST_ARR
    d["gdn_norm_g"] = rep(np.asarray(inp["gdn_norm_g"], f32)[0])
    d["mlstm_norm_g"] = rep(np.asarray(inp["mlstm_norm_g"], f32)[0])
    return {k: np.ascontiguousarray(v, dtype=f32) for k, v in d.items()}


def prep_core(inp, b, half, S, shared=None):
    f32 = np.float32
    x = np.asarray(inp["x"], f32)[b][:S]
    xl = x[::-1] if half == 0 else x
    df, db = (0, 1) if half == 1 else (1, 0)
    S_OWN = S // 2
    xT = np.zeros((1024, S + 4), f32)
    xT[:, 2:S + 2] = xl.T
    w_in = np.asarray(inp["w_in"], f32)[0]
    sizes = [512, 512, 512, 512, 8, 8, 512, 512, 512, 512, 8, 8]
    offs = np.cumsum([0] + sizes)
    gq, gk, gv, gz, ga, gb, mq, mk, mv, mo, mi, mf = [w_in[:, offs[i]:offs[i + 1]] for i in range(12)]

    def dsel(w):
        return np.concatenate([w[:, 4 * df:4 * df + 4], w[:, 4 * db:4 * db + 4]], axis=1)
    gates = np.concatenate([dsel(ga), dsel(gb), dsel(mi), dsel(mf)], axis=1)
    w_fm = np.concatenate([gq, gk, gv, mq], axis=1)
    w_tm = np.concatenate([gz, mo, mv, mk, gates], axis=1)
    cw = np.asarray(inp["conv_w"], f32)[0]
    if half == 0:
        cw = cw[:, ::-1]
    convw = cw.reshape(12, 128, 5).transpose(1, 0, 2).reshape(128, 60)
    n1g = np.asarray(inp["norm1_g"], f32)[0].reshape(8, 128).T

    def rep(v):
        return np.repeat(np.asarray(v, f32).reshape(1, -1), 128, axis=0)

    def dvec(p):
        p = np.asarray(p, f32)[0]
        return np.concatenate([p[df], p[db]])
    gp_add = np.concatenate([dvec(inp["gdn_dt_bias"]), np.zeros(8, f32), dvec(inp["mlstm_i_bias"]), dvec(inp["mlstm_f_bias"])])
    d = dict(
        xT=xT, x_own=np.ascontiguousarray(xl[S - S_OWN:]),
        w_in_fm=np.ascontiguousarray(w_fm), w_in_tm=np.ascontiguousarray(w_tm),
        conv_w=np.ascontiguousarray(convw),
        norm1_g=np.ascontiguousarray(n1g), gp_add=rep(gp_add), alog=rep(dvec(inp["gdn_a_log"])),
    )
    if shared is None:
        shared = prep_shared(inp)
    d = {k: np.ascontiguousarray(v, dtype=f32) for k, v in d.items()}
    d.update(shared)
    return d


_NC_CACHE = {}


def kernel(**inputs):
    from concourse.bass_utils import run_bass_kernel_spmd
    x = np.asarray(inputs["x"])
    B, S, D = x.shape
    NTH = S // 256
    key = (NTH,)
    if key not in _NC_CACHE:
        _NC_CACHE[key] = build(NTH, NTH, phase2=True)
    nc = _NC_CACHE[key]
    shared = prep_shared(inputs)
    maps = []
    for c in range(8):
        maps.append(prep_core(inputs, c // 2, c % 2, S, shared))
    res = run_bass_kernel_spmd(nc, maps, core_ids=list(range(8)))
    out = np.empty((B, S, D), np.float32)
    for c in range(8):
        b, half = c // 2, c % 2
        o = res.results[c]["out"]
        if half == 0:
            out[b, :S // 2] = o[::-1]
        else:
            out[b, S // 2:] = o
    return out
```

```python
import numpy as np
from contextlib import ExitStack
import concourse.bass as bass
import concourse.mybir as mybir

F32 = mybir.dt.float32; BF16 = mybir.dt.bfloat16; U32 = mybir.dt.uint32; I32 = mybir.dt.int32
AF = mybir.ActivationFunctionType; ALU = mybir.AluOpType; AX = mybir.AxisListType

class Buf:
    __slots__ = ("name", "w", "r", "dsem", "dcnt", "t")
    def __init__(self, name, t=None):
        self.name = name; self.w = None; self.r = {}; self.dsem = None; self.dcnt = 0; self.t = t
    def __getitem__(self, k):
        return self.t[k]

ENGS = ("pe", "dve", "act", "pool", "sp")

class Prog:
    def __init__(self, nc, same_sync=("dve", "act", "pool")):
        self.nc = nc
        self.es = ExitStack()
        self.sem = {}
        for e in ENGS:
            self.sem[e] = self.es.enter_context(nc.semaphore("prog_" + e))
        self.count = {e: 0 for e in ENGS}
        self.waited = {e: {} for e in ENGS}
        self.prog = {e: [] for e in ENGS}
        self.same_sync = set(same_sync)
        self.nbuf = 0
        self.ndsem = 0

    def _arena(self):
        if getattr(self, "arena", None) is None:
            nbytes = (self.nc.sbuf_bytes_remaining - 2048) // 64 * 64
            self.arena_words = nbytes // 4
            self.arena = self.es.enter_context(self.nc.sbuf_tensor("arena", [128, self.arena_words], F32))
            self.aoff = 0
            self.dbufs = []
        return self.arena

    def sb(self, name, shape, dt):
        ar = self._arena()
        shape = list(shape)
        assert shape[0] == 128
        nelem = 1
        for s_ in shape[1:]:
            nelem *= s_
        esz = {F32: 4, BF16: 2, U32: 4, I32: 4}[dt]
        nwords = (nelem * esz + 63) // 64 * 16
        assert self.aoff + nwords <= self.arena_words, ("SBUF arena overflow", name, self.aoff * 4, nwords * 4)
        ap = ar[:, self.aoff:self.aoff + nwords]
        self.aoff += nwords
        if dt != F32:
            ap = ap.bitcast(dt)
        ap = ap[:, 0:nelem]
        if len(shape) == 3:
            ap = ap.rearrange("p (a b) -> p a b", a=shape[1])
        elif len(shape) == 4:
            ap = ap.rearrange("p (a b c) -> p a b c", a=shape[1], b=shape[2])
        return Buf(name, ap)

    def mark(self):
        self._arena()
        return self.aoff

    def release(self, mark):
        self.aoff = mark

    def ps(self, name, shape, dt=F32):
        t = self.es.enter_context(self.nc.psum_tensor("p_" + name, list(shape), dt))
        return Buf(name, t)
    def view(self, name, t):
        return Buf(name, t)

    def _need(self, eng, waits, ev):
        if ev is None:
            return
        key, val = ev
        if key == eng and eng not in self.same_sync:
            return
        if self.waited[eng].get(key, 0) >= val:
            return
        if waits.get(key, 0) < val:
            waits[key] = val

    def _deps(self, eng, reads, writes):
        waits = {}
        for b in reads:
            self._need(eng, waits, b.w)
        for b in writes:
            self._need(eng, waits, b.w)
            for s, v in b.r.items():
                self._need(eng, waits, (s, v))
        for s, v in waits.items():
            self.waited[eng][s] = v
        return waits

    def _mark(self, ev, reads, writes):
        s, v = ev
        for b in reads:
            if b.r.get(s, 0) < v:
                b.r[s] = v
        for b in writes:
            b.w = ev
            b.r = {}

    def op(self, eng, fn, reads=(), writes=()):
        waits = self._deps(eng, reads, writes)
        self.count[eng] += 1
        ev = (eng, self.count[eng])
        self._mark(ev, reads, writes)
        self.prog[eng].append((waits, fn, eng, self.count[eng]))
        return ev

    def dma(self, eng, fn, dbuf, reads=(), writes=()):
        waits = self._deps(eng, reads, writes)
        if dbuf.dsem is None:
            dbuf.dsem = self.es.enter_context(self.nc.semaphore("d_" + dbuf.name))
            self.ndsem += 1
            self._arena()
            self.dbufs.append(dbuf)
        dbuf.dcnt += 16
        ev = (dbuf.dsem, dbuf.dcnt)
        self._mark(ev, reads, writes)
        self.prog[eng].append((waits, fn, dbuf.dsem, 16))
        return ev

    def wait_all(self, eng, bufs):
        waits = {}
        for b in bufs:
            self._need(eng, waits, b.w)
            for s, v in b.r.items():
                self._need(eng, waits, (s, v))
        for s, v in waits.items():
            self.waited[eng][s] = v
        self.prog[eng].append((waits, None, None, 0))

    def barrier(self):
        evs = [(e, self.count[e]) for e in ENGS if self.count[e] > 0]
        evs += [(b.dsem, b.dcnt) for b in self.dbufs]
        for e in ENGS:
            waits = {}
            for ev in evs:
                self._need(e, waits, ev)
            for s_, v in waits.items():
                self.waited[e][s_] = v
            self.prog[e].append((waits, None, None, 0))

    def emit(self):
        nc = self.nc
        sig = {e: set() for e in ENGS}
        for e in ENGS:
            for waits, fn, key, val in self.prog[e]:
                for k, v in waits.items():
                    if isinstance(k, str):
                        sig[k].add(v)
        rank = {}
        for e in ENGS:
            rank[e] = {idx: i + 1 for i, idx in enumerate(sorted(sig[e]))}
        self.nsig = {e: len(sig[e]) for e in ENGS}
        with nc.Block() as block:
            def run(e, engine):
                for waits, fn, key, val in self.prog[e]:
                    for k, v in waits.items():
                        if isinstance(k, str):
                            engine.wait_ge(self.sem[k], rank[k][v])
                        else:
                            engine.wait_ge(k, v)
                    if fn is not None:
                        ins = fn(engine)
                        if isinstance(key, str):
                            if val in rank[key]:
                                ins.then_inc(self.sem[key], 1)
                        else:
                            ins.then_inc(key, val)
            @block.tensor
            def _(engine): run("pe", engine)
            @block.vector
            def _(engine): run("dve", engine)
            @block.scalar
            def _(engine): run("act", engine)
            @block.gpsimd
            def _(engine): run("pool", engine)
            @block.sync
            def _(engine): run("sp", engine)
        self.es.close()


def _bufs(*vs):
    out = []
    for v in vs:
        if isinstance(v, tuple):
            out.append(v[0])
    return out

class Ops:
    def __init__(self, P):
        self.P = P

    def mm(self, out, lhsT, rhs, start=True, stop=True):
        o, l, r = out[1], lhsT[1], rhs[1]
        self.P.op("pe", lambda e: e.matmul(o, lhsT=l, rhs=r, start=start, stop=stop),
                  reads=_bufs(lhsT, rhs), writes=_bufs(out))

    def tr(self, out, in_, ident):
        o, i, d = out[1], in_[1], ident[1]
        self.P.op("pe", lambda e: e.transpose(o, i, d), reads=_bufs(in_, ident), writes=_bufs(out))

    def act(self, out, in_, func, scale=1.0, bias=None, accum=None):
        o, i = out[1], in_[1]
        kw = {}
        rd = _bufs(in_)
        wr = _bufs(out)
        if bias is not None:
            if isinstance(bias, tuple):
                kw["bias"] = bias[1]; rd += [bias[0]]
            else:
                kw["bias"] = bias
        if isinstance(scale, tuple):
            kw["scale"] = scale[1]; rd += [scale[0]]
        else:
            kw["scale"] = scale
        if accum is not None:
            kw["accum_out"] = accum[1]; wr += [accum[0]]
        self.P.op("act", lambda e: e.activation(out=o, in_=i, func=func, **kw), reads=rd, writes=wr)

    def cp(self, out, in_, eng="act"):
        o, i = out[1], in_[1]
        if eng == "act":
            self.P.op("act", lambda e: e.copy(out=o, in_=i), reads=_bufs(in_), writes=_bufs(out))
        else:
            self.P.op(eng, lambda e: e.tensor_copy(out=o, in_=i), reads=_bufs(in_), writes=_bufs(out))

    def tt(self, out, a, b, op, eng="dve"):
        o, x, y = out[1], a[1], b[1]
        self.P.op(eng, lambda e: e.tensor_tensor(out=o, in0=x, in1=y, op=op), reads=_bufs(a, b), writes=_bufs(out))

    def ts(self, out, a, s1, s2=None, op0=ALU.mult, op1=ALU.bypass, eng="dve", accum=None):
        o, x = out[1], a[1]
        rd = _bufs(a, s1, s2)
        wr = _bufs(out)
        v1 = s1[1] if isinstance(s1, tuple) else s1
        v2 = s2[1] if isinstance(s2, tuple) else s2
        kw = {}
        if accum is not None:
            kw["accum_out"] = accum[1]; wr += [accum[0]]
        self.P.op(eng, lambda e: e.tensor_scalar(out=o, in0=x, scalar1=v1, scalar2=v2, op0=op0, op1=op1, **kw), reads=rd, writes=wr)

    def stt(self, out, a, scalar, b, op0, op1, accum=None):
        o, x, y = out[1], a[1], b[1]
        rd = _bufs(a, b, scalar)
        wr = _bufs(out)
        sv = scalar[1] if isinstance(scalar, tuple) else scalar
        kw = {}
        if accum is not None:
            kw["accum_out"] = accum[1]; wr += [accum[0]]
        self.P.op("dve", lambda e: e.scalar_tensor_tensor(out=o, in0=x, scalar=sv, in1=y, op0=op0, op1=op1, **kw), reads=rd, writes=wr)

    def red(self, out, in_, op, axis=AX.X, eng="dve"):
        o, i = out[1], in_[1]
        self.P.op(eng, lambda e: e.tensor_reduce(out=o, in_=i, axis=axis, op=op), reads=_bufs(in_), writes=_bufs(out))

    def recip(self, out, in_):
        o, i = out[1], in_[1]
        self.P.op("dve", lambda e: e.reciprocal(out=o, in_=i), reads=_bufs(in_), writes=_bufs(out))

    def memset(self, out, val, eng="dve"):
        o = out[1]
        self.P.op(eng, lambda e: e.memset(o, val), writes=_bufs(out))

    def dma(self, out, in_, eng="sp", dbuf=None):
        o = out[1] if isinstance(out, tuple) else out
        i = in_[1] if isinstance(in_, tuple) else in_
        rd = _bufs(in_); wr = _bufs(out)
        db = dbuf if dbuf is not None else (wr[0] if wr else rd[0])
        self.P.dma(eng, lambda e: e.dma_start(out=o, in_=i), db, reads=rd, writes=wr)


import numpy as np
import concourse.bass as bass
import concourse.mybir as mybir

NEG = -30000.0
EPS = 1e-6


def bc_last(ap, n):
    return ap.unsqueeze(2).to_broadcast([ap.shape[0], ap.shape[1], n])


def bc_mid(ap, n):
    return ap.unsqueeze(1).to_broadcast([ap.shape[0], n, ap.shape[1]])


def build_consts():
    i = np.arange(128)
    blk = (i[:, None] // 64) == (i[None, :] // 64)
    LT = ((i[None, :] <= i[:, None]) & blk).astype(np.float32)
    UT = ((i[None, :] >= i[:, None]) & blk).astype(np.float32)
    SL = ((i[None, :] < i[:, None]) & blk).astype(np.float32)
    SU = ((i[None, :] > i[:, None]) & blk).astype(np.float32)
    ident = np.eye(128, dtype=np.float32)
    ones = np.ones((128, 128), np.float32)
    blk0 = np.repeat((i < 64).astype(np.float32)[:, None], 128, 1)
    blk1 = np.repeat((i >= 64).astype(np.float32)[:, None], 128, 1)
    mats = dict(LT=LT, UT=UT, SL=SL, SU=SU, ident=ident, ones=ones, blk0=blk0, blk1=blk1,
                NSL=NEG * (1 - SL), NSU=NEG * (1 - SU), NUT=NEG * (1 - UT), NLT=NEG * (1 - LT))
    names = list(mats.keys())
    arr = np.concatenate([mats[k] for k in names], axis=1).astype(np.float32)
    return names, arr


CONST_NAMES, CONST_ARR = build_consts()


def build(NT_OTHER, NT_OWN, phase2=False, debug=False):
    nc = bass.Bass("TRN2", target_bir_lowering=False)
    NT = NT_OTHER + NT_OWN
    S = 128 * NT
    S_OWN = 128 * NT_OWN

    def din(name, shape, dt=F32):
        return nc.dram_tensor(name, list(shape), dt, kind="ExternalInput").ap()

    xT_d = din("xT", [1024, S + 4])
    xown_d = din("x_own", [S_OWN, 1024])
    wfm_d = din("w_in_fm", [1024, 2048])
    wtm_d = din("w_in_tm", [1024, 2080])
    wout_d = din("w_out", [1024, 1024])
    consts_d = din("consts", [128, len(CONST_NAMES) * 128])
    convw_d = din("conv_w", [128, 60])
    n1g_d = din("norm1_g", [128, 8])
    gpadd_d = din("gp_add", [128, 32])
    alog_d = din("alog", [128, 8])
    gng_d = din("gdn_norm_g", [128, 128])
    mng_d = din("mlstm_norm_g", [128, 512])
    wq_d = din("wq", [1024, 2048])
    keysT_d = din("keysT", [128, 2048])
    n2g_d = din("n2g", [128, 1024])
    nfg_d = din("nfg", [128, 1024])
    UT_d = din("UT", [128, 128 * 1024])
    V_d = din("Vt", [128, 128 * 1024])
    iot_d = din("iotas", [128, 144])
    x1_d = nc.dram_tensor("x1_scr", [NT_OWN, 128, 1024], F32, kind="Internal").ap()
    UTs_d = nc.dram_tensor("UT_bf", [128, 128 * 1024], BF16, kind="Internal").ap()
    Vs_d = nc.dram_tensor("V_bf", [128, 128 * 1024], BF16, kind="Internal").ap()
    out_d = nc.dram_tensor("out", [S_OWN, 1024], F32, kind="ExternalOutput").ap()
    ob_d = nc.dram_tensor("ob_scr", [NT_OWN, 128, 1024], F32, kind="Internal").ap()

    P = Prog(nc)
    O = Ops(P)

    consts = P.sb("consts", [128, len(CONST_NAMES) * 128], F32)
    O.dma((consts, consts.t[:, :]), consts_d)
    C = {k: (consts, consts.t[:, i * 128:(i + 1) * 128]) for i, k in enumerate(CONST_NAMES)}
    identb = P.sb("identb", [128, 128], BF16)
    O.cp((identb, identb.t[:, :]), C["ident"], eng="dve")

    iot = P.sb("iot", [128, 144], F32)
    O.dma((iot, iot.t[:, :]), iot_d)
    PH_MARK = P.mark()
    wfm = P.sb("wfm", [128, 8, 2048], BF16)
    wtm = P.sb("wtm", [128, 8, 2080], BF16)
    wout = P.sb("wout", [128, 8, 1024], BF16)
    for j in range(8):
        O.dma((wfm, wfm.t[:, j, :]), wfm_d[j * 128:(j + 1) * 128, :], eng="pool")
        O.dma((wtm, wtm.t[:, j, :]), wtm_d[j * 128:(j + 1) * 128, :], eng="pool")
        O.dma((wout, wout.t[:, j, :]), wout_d[j * 128:(j + 1) * 128, :], eng="pool")
    UTs = P.view("UTs", UTs_d)
    Vs = P.view("Vs", Vs_d)
    CH = 8192

    def conv_chunk(cix):
        O.dma((UTs, UTs_d[:, cix * CH:(cix + 1) * CH]), UT_d[:, cix * CH:(cix + 1) * CH], eng="pool")
        O.dma((Vs, Vs_d[:, cix * CH:(cix + 1) * CH]), V_d[:, cix * CH:(cix + 1) * CH], eng="pool")
    n_conv = 131072 // CH
    convw = P.sb("convw", [128, 12, 5], F32)
    O.dma((convw, convw.t[:, :, :].rearrange("p c k -> p (c k)")), convw_d)
    n1g = P.sb("n1g", [128, 8], F32)
    O.dma((n1g, n1g.t[:, :]), n1g_d)
    gpadd = P.sb("gpadd", [128, 32], F32)
    O.dma((gpadd, gpadd.t[:, :]), gpadd_d)
    alog = P.sb("alog", [128, 8], F32)
    O.dma((alog, alog.t[:, :]), alog_d)
    for j in range(8):
        O.act((wfm, wfm.t[:, j, :]), (wfm, wfm.t[:, j, :]), AF.Copy, scale=(n1g, n1g.t[:, j:j + 1]))
        O.act((wtm, wtm.t[:, j, :]), (wtm, wtm.t[:, j, :]), AF.Copy, scale=(n1g, n1g.t[:, j:j + 1]))
    nEa = P.sb("nEa", [128, 8], F32)
    O.act((nEa, nEa.t[:, :]), (alog, alog.t[:, :]), AF.Exp)
    O.ts((nEa, nEa.t[:, :]), (nEa, nEa.t[:, :]), -1.0, op0=ALU.mult)
    gng = P.sb("gng", [128, 4, 128], F32)
    for h in range(4):
        O.dma((gng, gng.t[:, h, :]), gng_d)
    mng = P.sb("mng", [128, 4, 128], F32)
    O.dma((mng, mng.t[:, :, :].rearrange("p h d -> p (h d)")), mng_d)

    banks = [P.ps("bank%d" % i, [128, 512], F32) for i in range(8)]
    bank_i = [0]

    def pbank():
        b = banks[bank_i[0] % 8]
        bank_i[0] += 1
        return b

    def pv4(b):
        return b.t[:, :].rearrange("p (h s) -> p h s", h=4)

    xt = [P.sb("xt0", [128, 8, 132], F32)]
    rstd = P.sb("rstd", [128, 132], F32)
    hT = P.sb("hT", [128, 8, 132], BF16)
    pre = P.sb("pre", [128, 12, 132], F32)
    cacc = P.sb("cacc", [128, 12, 128], F32)
    ctmp = P.sb("ctmp", [128, 4, 128], F32)
    nsq = ctmp
    cbufs = [P.sb("cbuf%d" % i, [128, 4, 128], BF16) for i in range(2)]
    nrs = ctmp
    gqT2 = [P.sb("gqT%d" % i, [128, 4, 128], BF16) for i in range(2)]
    gkT2 = [P.sb("gkT%d" % i, [128, 4, 128], BF16) for i in range(2)]
    mqT2 = [P.sb("mqT%d" % i, [128, 4, 128], BF16) for i in range(2)]
    mkT2 = [P.sb("mkT%d" % i, [128, 4, 128], BF16) for i in range(2)]
    mvx3 = [P.sb("mvx%d" % i, [128, 4, 130], BF16) for i in range(3)]
    for i in range(3):
        O.memset((mvx3[i], mvx3[i].t[:, :, :]), 1.0)
    tot3 = [P.sb("tot%d" % i, [128, 16], F32) for i in range(3)]
    mkt2 = [P.sb("mkt%d" % i, [128, 4, 128], F32) for i in range(2)]
    ktok2 = [P.sb("ktok%d" % i, [128, 4, 128], F32) for i in range(2)]
    vtok2 = [P.sb("vtok%d" % i, [128, 4, 128], F32) for i in range(2)]
    gpre2 = [P.sb("gpre%d" % i, [128, 32], F32) for i in range(2)]
    gsm_2 = [P.sb("gsm%d" % i, [128, 16], F32) for i in range(2)]
    gcat2 = [P.sb("gcat%d" % i, [128, 8], F32) for i in range(2)]
    sm2_2 = [P.sb("sm2%d" % i, [128, 32], F32) for i in range(2)]
    SLg = P.sb("SLg", [128, 4, 128], F32)
    UTg = P.sb("UTg", [128, 4, 128], F32)
    Dm = P.sb("Dm", [128, 4, 128], F32)
    A0 = P.sb("A0", [128, 4, 128], F32)
    A0T = P.sb("A0T", [128, 4, 128], F32)
    Mb0 = P.sb("Mb0", [128, 4, 128], F32)
    MTb0 = P.sb("MTb0", [128, 4, 128], F32)
    Pm = P.sb("Pm", [128, 4, 128], F32)
    TTb = P.sb("TTb", [128, 4, 128], BF16)
    vb = P.sb("vb", [128, 4, 128], BF16)
    kbg = P.sb("kbg", [128, 4, 128], BF16)
    kdec_2 = [P.sb("kdec%d" % i, [128, 4, 128], BF16) for i in range(2)]
    u_sb_2 = [P.sb("u_sb%d" % i, [128, 4, 128], F32) for i in range(2)]
    wT_2 = [P.sb("wT%d" % i, [128, 4, 128], BF16) for i in range(2)]
    qkT_2 = [P.sb("qkT%d" % i, [128, 4, 128], BF16) for i in range(2)]
    qdT_2 = [P.sb("qdT%d" % i, [128, 4, 128], BF16) for i in range(2)]
    Eg = Dm
    vnew = P.sb("vnew", [128, 4, 128], BF16)
    kw_2 = [P.sb("kw%d" % i, [128, 4, 128], BF16) for i in range(2)]
    pT_2 = [P.sb("pT%d" % i, [128, 4, 128], BF16) for i in range(2)]
    mqd_2 = [P.sb("mqd%d" % i, [128, 4, 128], BF16) for i in range(2)]
    o_sb = P.sb("o_sb", [128, 8, 128], F32)
    ob_sb = P.sb("ob_sb", [128, 8, 128], F32)
    y_sb = P.sb("y_sb", [128, 8, 128], BF16)
    yT = P.sb("yT", [128, 8, 128], BF16)
    zs3 = [P.sb("zs%d" % i, [128, 8, 128], BF16) for i in range(3)]
    xtok = P.sb("xtok", [128, 1024], F32)
    x1 = xtok
    Sg = P.sb("Sg", [128, 4, 128], F32)
    Sgb = P.sb("Sgb", [128, 4, 128], BF16)
    Cm = P.sb("Cm", [128, 4, 130], F32)
    Cmb = P.sb("Cmb", [128, 4, 130], BF16)
    gsm2 = P.sb("gsm2", [128, 16], F32)
    negb = P.sb("negb", [128, 2, 128], BF16)
    negb_dir = [None]

    def reset_states():
        O.memset((Sg, Sg.t), 0.0)
        O.memset((Sgb, Sgb.t), 0.0)
        O.memset((Cm, Cm.t), 0.0)
        O.memset((Cmb, Cmb.t), 0.0)
    gpool = [banks[0], banks[1], banks[2]]
    mpool = [banks[4], banks[5], banks[6]]
    fpool = [banks[3], banks[7]]
    pst = {"g": 0, "m": 0, "f": 0, "gpin": set(), "mpin": set(), "fpin": set()}

    def falloc():
        return _alloc(fpool, "f", "fpin", False)

    def _alloc(pool, key, pinkey, pin):
        for _ in range(8):
            b = pool[pst[key] % len(pool)]
            pst[key] += 1
            if b.name not in pst[pinkey]:
                if pin:
                    pst[pinkey].add(b.name)
                return b
        raise RuntimeError("no free psum bank")

    def galloc(pin=False):
        return _alloc(gpool, "g", "gpin", pin)

    def malloc_(pin=False):
        return _alloc(mpool, "m", "mpin", pin)

    def gunpin(b):
        pst["gpin"].discard(b.name)

    def munpin(b):
        pst["mpin"].discard(b.name)
    ob_bufs = [P.view("ob_d%d" % i, ob_d[i]) for i in range(NT_OWN)]
    x1_bufs = [P.view("x1_d%d" % i, x1_d[i]) for i in range(NT_OWN)]
    out_bufs = [P.view("out_d%d" % i, out_d[i * 128:(i + 1) * 128, :]) for i in range(NT_OWN)]

    DIRM = [dict(Linc="UT", Rem="SL", NA="NSL", NT="NUT"),
            dict(Linc="LT", Rem="SU", NA="NSU", NT="NLT")]

    def V(b, ap=None):
        return (b, b.t if ap is None else ap)

    load_i = [0]

    def front_steps(ti, d, with_out, final, kk):
        par = kk % 2
        dm = DIRM[d]
        Linc, Rem = C[dm["Linc"]], C[dm["Rem"]]
        I_f, ones = C["ident"], C["ones"]
        gqT, gkT, mqT, mkT, mvx = gqT2[par], gkT2[par], mqT2[par], mkT2[par], mvx3[kk % 3]
        ktok, vtok, mkt, zs = ktok2[par], vtok2[par], mkt2[par], zs3[kk % 3]
        gpre, gcat, gsm, sm2 = gpre2[par], gcat2[par], gsm_2[par], sm2_2[par]
        tot_b = tot3[kk % 3]
        xb = xt[0]
        F = []
        fm_groups = [1, 2] if not with_out else [0, 1, 2]

        def f_norm():
            src = xT_d[:, ti * 128: ti * 128 + 132].rearrange("(j p) n -> p j n", p=128)
            O.dma((xb, xb.t[:, :, :]), src)
            sqv = pre.t[:, 0:8, :]
            O.act((pre, sqv), (xb, xb.t[:, :, :]), AF.Square)
        F.append(f_norm)

        def f_norm2():
            bk = falloc()
            for j in range(8):
                O.mm((bk, bk.t[:, 0:132]), ones, (pre, pre.t[:, j, :]), start=(j == 0), stop=(j == 7))
            O.act((rstd, rstd.t[:, :]), (bk, bk.t[:, 0:132]), AF.Ln, scale=1.0 / 1024, bias=EPS)
            O.act((rstd, rstd.t[:, :]), (rstd, rstd.t[:, :]), AF.Exp, scale=-0.5)
            O.tt((hT, hT.t), (xb, xb.t), (rstd, bc_mid(rstd.t, 8)), ALU.mult)
        F.append(f_norm2)
        chs = [g * 4 + c for g in fm_groups for c in range(4)]
        for c0 in range(0, len(chs), 3):
            def f_fm(c0=c0):
                grp = chs[c0:c0 + 3]
                bk = falloc()
                for cc, ch in enumerate(grp):
                    for j in range(8):
                        O.mm((bk, bk.t[:, cc * 132:(cc + 1) * 132]), (wfm, wfm.t[:, j, ch * 128:(ch + 1) * 128]),
                             (hT, hT.t[:, j, :]), start=(j == 0), stop=(j == 7))
                n = len(grp)
                O.cp((pre, pre.t[:, grp[0]:grp[0] + n, :]),
                     (bk, bk.t[:, 0:132 * n].rearrange("p (c n) -> p c n", c=n)), eng="act")
            F.append(f_fm)
        cst = {}
        for g in fm_groups:
            for k in range(5):
                def f_conv(g=g, k=k):
                    if k == 0:
                        cst[g] = falloc()
                    bk = cst[g]
                    w_bc = (convw, bc_last(convw.t[:, g * 4:(g + 1) * 4, k], 128))
                    src_k = (pre, pre.t[:, g * 4:(g + 1) * 4, k:k + 128])
                    ct = cbufs[(g * 5 + k) % 2]
                    O.tt(V(ct), src_k, w_bc, ALU.mult, eng="pool")
                    O.mm((bk, bk.t[:, :]), (identb, identb.t), (ct, ct.t.rearrange("p a b -> p (a b)")),
                         start=(k == 0), stop=(k == 4))
                    if k == 4:
                        acc = (cacc, cacc.t[:, g * 4:(g + 1) * 4, :])
                        O.act(acc, (bk, pv4(bk)), AF.Silu)
                F.append(f_conv)
        for g in [g for g in fm_groups if g < 2]:
            def f_l2(g=g):
                acc = (cacc, cacc.t[:, g * 4:(g + 1) * 4, :])
                O.act(V(nsq), acc, AF.Square)
            F.append(f_l2)

            def f_l2b(g=g):
                acc = (cacc, cacc.t[:, g * 4:(g + 1) * 4, :])
                bk = falloc()
                for h in range(4):
                    O.mm((bk, pv4(bk)[:, h, :]), ones, (nsq, nsq.t[:, h, :]))
                O.act(V(nrs), (bk, pv4(bk)), AF.Ln, bias=EPS)
                O.act(V(nrs), V(nrs), AF.Exp, scale=-0.5)
                if g == 0:
                    O.stt(V(gqT), acc, 128.0 ** -0.5, V(nrs), ALU.mult, ALU.mult)
                else:
                    O.tt(acc, acc, V(nrs), ALU.mult)
                    O.cp(V(gkT), acc, eng="pool")
            F.append(f_l2b)

        def f_tr():
            bk = falloc()
            for h in range(4):
                O.tr((bk, pv4(bk)[:, h, :]), (cacc, cacc.t[:, 4 + h, :]), I_f)
            O.cp(V(ktok), (bk, pv4(bk)), eng="act")
            bk = falloc()
            for h in range(4):
                O.tr((bk, pv4(bk)[:, h, :]), (cacc, cacc.t[:, 8 + h, :]), I_f)
            O.cp(V(vtok), (bk, pv4(bk)), eng="act")
        F.append(f_tr)

        def tm_proj0(c0, n):
            bk = falloc()
            for j in range(8):
                O.mm((bk, bk.t[:, 0:n]), (hT, hT.t[:, j, 2:130]), (wtm, wtm.t[:, j, c0:c0 + n]),
                     start=(j == 0), stop=(j == 7))
            return bk

        def f_gates():
            bk = tm_proj0(2048, 32)
            O.tt((gpre, gpre.t[:, :]), (bk, bk.t[:, 0:32]), (gpadd, gpadd.t[:, :]), ALU.add)
            a_ = (gpre, gpre.t[:, 4 * d:4 * d + 4])
            b_ = (gpre, gpre.t[:, 8 + 4 * d:12 + 4 * d])
            i_ = (gpre, gpre.t[:, 16 + 4 * d:20 + 4 * d])
            f_ = (gpre, gpre.t[:, 24 + 4 * d:28 + 4 * d])
            g_ = (gcat, gcat.t[:, 0:4])
            lf_ = (gcat, gcat.t[:, 4:8])
            t0 = (gsm, gsm.t[:, 0:4]); t1 = (gsm, gsm.t[:, 4:8])
            beta = (gsm, gsm.t[:, 8:12])
            O.act(t0, a_, AF.Exp)
            O.act(t1, f_, AF.Exp, scale=-1.0)
            O.act(beta, b_, AF.Exp, scale=-1.0)
            t012 = (gsm, gsm.t[:, 0:12])
            O.act(t012, t012, AF.Ln, bias=1.0)
            O.tt(g_, t0, (nEa, nEa.t[:, 4 * d:4 * d + 4]), ALU.mult)
            O.ts(lf_, t1, -1.0, op0=ALU.mult)
            O.act(beta, beta, AF.Exp, scale=-1.0)
        F.append(f_gates)

        def f_gates2():
            i_ = (gpre, gpre.t[:, 16 + 4 * d:20 + 4 * d])
            beta = (gsm, gsm.t[:, 8:12])
            bk = falloc()
            gcat_v = (gcat, gcat.t[:, :])
            O.mm((bk, bk.t[:, 0:8]), Linc, gcat_v)
            O.mm((bk, bk.t[:, 8:16]), Rem, gcat_v)
            O.mm((bk, bk.t[:, 16:24]), C["blk0"], gcat_v)
            O.mm((bk, bk.t[:, 24:32]), C["blk1"], gcat_v)
            egc = (sm2, sm2.t[:, 0:4]); kbgs = (sm2, sm2.t[:, 4:8]); kdcs = (sm2, sm2.t[:, 8:12])
            kws = (sm2, sm2.t[:, 12:16]); tot = (tot_b, tot_b.t[:, 0:16])
            O.act((sm2, sm2.t[:, 0:12]), (bk, bk.t[:, 0:12]), AF.Exp)
            O.tt(kbgs, egc, beta, ALU.mult)
            O.tt(kws, (bk, bk.t[:, 12:16]), i_, ALU.add)
            O.act(kws, kws, AF.Exp)
            O.act(tot, (bk, bk.t[:, 16:32]), AF.Exp)
        F.append(f_gates2)
        if with_out:
            def f_mq():
                bk = falloc()
                for h in range(4):
                    ch = 12 + h
                    for j in range(8):
                        O.mm((bk, pv4(bk)[:, h, :]), (wfm, wfm.t[:, j, ch * 128:(ch + 1) * 128]),
                             (hT, hT.t[:, j, 2:130]), start=(j == 0), stop=(j == 7))
                O.cp(V(mqT), (bk, pv4(bk)), eng="act")
            F.append(f_mq)

        def f_mv():
            bk = tm_proj0(1024, 512)
            O.cp((mvx, mvx.t[:, :, 0:128]), (bk, pv4(bk)), eng="act")
        F.append(f_mv)

        def f_mk():
            bk = tm_proj0(1536, 512)
            O.act(V(mkt), (bk, pv4(bk)), AF.Copy, scale=128.0 ** -0.5)
        F.append(f_mk)

        def f_mk2():
            if with_out:
                bk2 = falloc()
                for h in range(4):
                    O.tr((bk2, pv4(bk2)[:, h, :]), (mkt, mkt.t[:, h, :]), I_f)
                O.cp(V(mkT), (bk2, pv4(bk2)), eng="act")
        if with_out:
            F.append(f_mk2)
        if final:
            def f_z():
                bk_z = tm_proj0(0, 512)
                O.act((zs, zs.t[:, 0:4, :]), (bk_z, pv4(bk_z)), AF.Silu)
            def f_o():
                bk_o = tm_proj0(512, 512)
                O.act((zs, zs.t[:, 4:8, :]), (bk_o, pv4(bk_o)), AF.Sigmoid)
            F.append(f_z); F.append(f_o)
        return F

    def back_steps(ti, d, with_out, final, kk, first):
        par = kk % 2
        dm = DIRM[d]
        Linc, Rem, NA, NTm = C[dm["Linc"]], C[dm["Rem"]], C[dm["NA"]], C[dm["NT"]]
        I_f, ones = C["ident"], C["ones"]
        gqT, gkT, mqT, mkT, mvx = gqT2[par], gkT2[par], mqT2[par], mkT2[par], mvx3[kk % 3]
        ktok, vtok, mkt, zs = ktok2[par], vtok2[par], mkt2[par], zs3[kk % 3]
        gpre, gcat, gsm, sm2 = gpre2[par], gcat2[par], gsm_2[par], sm2_2[par]
        tot_b = tot3[kk % 3]
        kdec, u_sb, wT, qkT, qdT = kdec_2[par], u_sb_2[par], wT_2[par], qkT_2[par], qdT_2[par]
        kw, pT, mqd = kw_2[par], pT_2[par], mqd_2[par]
        SLf, UTf, Dmf = SLg, UTg, Dm
        idb = (identb, identb.t)
        NAb = (negb, negb.t[:, 0, :]); NTb = (negb, negb.t[:, 1, :])
        beta_bc = (gsm, bc_last(gsm.t[:, 8:12], 128))
        blks = (0, 1) if d == 0 else (1, 0)
        G = []

        def g1():
            if negb_dir[0] != d:
                negb_dir[0] = d
                O.cp(NAb, NA, eng="pool")
                O.cp(NTb, NTm, eng="pool")
            O.tt(V(SLg), (Rem[0], bc_mid(Rem[1], 4)), (gcat, bc_last(gcat.t[:, 0:4], 128)), ALU.mult, eng="pool")
            bkK = galloc(); bkD = galloc()
            for h in range(4):
                O.mm((bkK, pv4(bkK)[:, h, :]), (gkT, gkT.t[:, h, :]), (gkT, gkT.t[:, h, :]))
            for h in range(4):
                O.mm((bkD, pv4(bkD)[:, h, :]), Linc, (SLg, SLg.t[:, h, :]), start=True, stop=False)
                O.mm((bkD, pv4(bkD)[:, h, :]), idb, NAb, start=False, stop=True)
            O.act(V(Dm), (bkD, pv4(bkD)), AF.Exp)
            O.tt(V(A0), (bkK, pv4(bkK)), V(Dm), ALU.mult)
            O.tt(V(A0), V(A0), beta_bc, ALU.mult)
        G.append(g1)

        def g_sc():
            O.tt(V(vb), V(vtok), beta_bc, ALU.mult, eng="pool")
            O.tt(V(kbg), V(ktok), (sm2, bc_last(sm2.t[:, 4:8], 128)), ALU.mult, eng="pool")
            O.tt(V(kdec), V(ktok), (sm2, bc_last(sm2.t[:, 8:12], 128)), ALU.mult, eng="pool")

        def g2():
            bk = galloc()
            for h in range(4):
                O.tr((bk, pv4(bk)[:, h, :]), (A0, A0.t[:, h, :]), I_f)
            O.cp(V(A0T), (bk, pv4(bk)), eng="act")
            O.tt(V(Pm), (I_f[0], bc_mid(I_f[1], 4)), V(A0T), ALU.subtract)
        G.append(g2)
        chain = [(A0, A0T)]
        for lvl in range(5):
            chain.append((Mb0, MTb0) if lvl % 2 == 0 else (A0, A0T))
        def emit_sq(lvl):
            M, MT = chain[lvl]; M2, M2T = chain[lvl + 1]
            bk = galloc()
            for h in range(4):
                O.mm((bk, pv4(bk)[:, h, :]), (MT, MT.t[:, h, :]), (M, M.t[:, h, :]))
            bk2 = None
            if lvl < 4:
                bk2 = galloc()
                for h in range(4):
                    O.mm((bk2, pv4(bk2)[:, h, :]), (M, M.t[:, h, :]), (MT, MT.t[:, h, :]))
            return bk, bk2

        def evac_sq(lvl, bk, bk2):
            M2, M2T = chain[lvl + 1]
            O.cp(V(M2), (bk, pv4(bk)), eng="act")
            if bk2 is not None:
                O.cp(V(M2T), (bk2, pv4(bk2)), eng="act")

        def emit_pr(lvl):
            M2, M2T = chain[lvl + 1]
            bk3 = galloc()
            for h in range(4):
                O.mm((bk3, pv4(bk3)[:, h, :]), (M2, M2.t[:, h, :]), (Pm, Pm.t[:, h, :]))
            O.tt(V(Pm), V(Pm), (bk3, pv4(bk3)), ALU.add)

        def g_n0():
            bk, bk2 = emit_sq(0)
            evac_sq(0, bk, bk2)
        G.append(g_n0)
        for lvl in range(1, 5):
            def g_n(lvl=lvl):
                emit_pr(lvl - 1)
                bk, bk2 = emit_sq(lvl)
                evac_sq(lvl, bk, bk2)
            G.append(g_n)

        def g_n5():
            emit_pr(4)
        G.append(g_n5)

        def g3():
            O.cp(V(TTb), V(Pm), eng="act")
            bkU = galloc(); bkW = galloc()
            for h in range(4):
                O.mm((bkU, pv4(bkU)[:, h, :]), (TTb, TTb.t[:, h, :]), (vb, vb.t[:, h, :]))
            for h in range(4):
                O.mm((bkW, pv4(bkW)[:, h, :]), (kbg, kbg.t[:, h, :]), (TTb, TTb.t[:, h, :]))
            O.cp(V(u_sb), (bkU, pv4(bkU)), eng="act")
            O.cp(V(wT), (bkW, pv4(bkW)), eng="act")
        if with_out:
            def g_q1():
                O.tt(V(UTg), (Linc[0], bc_mid(Linc[1], 4)), (gcat, bc_last(gcat.t[:, 0:4], 128)), ALU.mult, eng="pool")
                bkQ = galloc(); bkD = galloc()
                for h in range(4):
                    O.mm((bkQ, pv4(bkQ)[:, h, :]), (gkT, gkT.t[:, h, :]), (gqT, gqT.t[:, h, :]))
                for h in range(4):
                    O.mm((bkD, pv4(bkD)[:, h, :]), (SLg, SLg.t[:, h, :]), Linc, start=True, stop=False)
                    O.mm((bkD, pv4(bkD)[:, h, :]), idb, NTb, start=False, stop=True)
                O.act(V(Dm), (bkD, pv4(bkD)), AF.Exp)
                O.tt(V(qkT), (bkQ, pv4(bkQ)), V(Dm), ALU.mult)

            def g_q2():
                bkG = galloc()
                for h in range(4):
                    O.mm((bkG, pv4(bkG)[:, h, :]), ones, (UTg, UTg.t[:, h, :]))
                O.act(V(Dm), (bkG, pv4(bkG)), AF.Exp)
                O.tt(V(qdT), V(gqT), V(Dm), ALU.mult)
            G.insert(3, g_q1)
            G.insert(5, g_q2)
        G.insert(2, g_sc)
        G.append(g3)
        S_, Sb_ = Sg, Sgb
        gst = {}
        for bi, blk in enumerate(blks):
            def g_blk(bi=bi, blk=blk):
                r0, r1 = blk * 64, blk * 64 + 64
                bkS = malloc_()
                for h in range(4):
                    O.mm((bkS, pv4(bkS)[r0:r1, h, :]), (wT, wT.t[:, h, r0:r1]), (Sb_, Sb_.t[:, h, :]))
                O.tt((vnew, vnew.t[r0:r1, :, :]), (u_sb, u_sb.t[r0:r1, :, :]), (bkS, pv4(bkS)[r0:r1, :, :]), ALU.subtract)
                if with_out:
                    bkO = malloc_()
                    for h in range(4):
                        O.mm((bkO, pv4(bkO)[r0:r1, h, :]), (qdT, qdT.t[:, h, r0:r1]), (Sb_, Sb_.t[:, h, :]), start=True, stop=False)
                        O.mm((bkO, pv4(bkO)[r0:r1, h, :]), (qkT, qkT.t[r0:r1, h, r0:r1]), (vnew, vnew.t[r0:r1, h, :]), start=False, stop=True)
                    O.cp((o_sb, o_sb.t[r0:r1, 0:4, :]), (bkO, pv4(bkO)[r0:r1, :, :]), eng="act")
                bkdS = malloc_()
                for h in range(4):
                    O.mm((bkdS, pv4(bkdS)[:, h, :]), (kdec, kdec.t[r0:r1, h, :]), (vnew, vnew.t[r0:r1, h, :]))
                O.tt(V(S_), V(S_), (tot_b, bc_last(tot_b.t[:, 8 * blk:8 * blk + 4], 128)), ALU.mult)
                O.tt(V(S_), V(S_), (bkdS, pv4(bkdS)), ALU.add)
                O.cp(V(Sb_), V(S_), eng="act")
            G.append(g_blk)

        Mx = []

        def m_kw():
            O.tt(V(kw), V(mkt), (sm2, bc_last(sm2.t[:, 12:16], 128)), ALU.mult, eng="pool")
        Mx.append(m_kw)
        if with_out:
            def m_p():
                O.tt(V(SLf), (Rem[0], bc_mid(Rem[1], 4)), (gcat, bc_last(gcat.t[:, 4:8], 128)), ALU.mult, eng="pool")
                bkQ = galloc(); bkD = galloc()
                for h in range(4):
                    O.mm((bkQ, pv4(bkQ)[:, h, :]), (mkT, mkT.t[:, h, :]), (mqT, mqT.t[:, h, :]))
                for h in range(4):
                    O.mm((bkD, pv4(bkD)[:, h, :]), (SLf, SLf.t[:, h, :]), Linc, start=True, stop=False)
                    O.mm((bkD, pv4(bkD)[:, h, :]), idb, NTb, start=False, stop=True)
                for h in range(4):
                    O.act((Dmf, Dmf.t[:, h, :]), (bkD, pv4(bkD)[:, h, :]), AF.Exp, bias=(gpre, gpre.t[:, 16 + 4 * d + h:17 + 4 * d + h]))
                O.tt(V(pT), (bkQ, pv4(bkQ)), V(Dmf), ALU.mult)
            Mx.append(m_p)

            def m_qd():
                O.tt(V(UTf), (Linc[0], bc_mid(Linc[1], 4)), (gcat, bc_last(gcat.t[:, 4:8], 128)), ALU.mult, eng="pool")
                bkG = galloc()
                for h in range(4):
                    O.mm((bkG, pv4(bkG)[:, h, :]), ones, (UTf, UTf.t[:, h, :]))
                O.act(V(Dmf), (bkG, pv4(bkG)), AF.Exp)
                O.tt(V(mqd), V(mqT), V(Dmf), ALU.mult)
            Mx.append(m_qd)
        C_, Cb_ = Cm, Cmb
        mst = {}
        for bi, blk in enumerate(blks):
            def m_blk(bi=bi, blk=blk):
                r0, r1 = blk * 64, blk * 64 + 64
                if with_out:
                    bkN = [malloc_(), malloc_()]
                    for h in range(4):
                        o_ap = bkN[h // 2].t[r0:r1, (h % 2) * 130:(h % 2) * 130 + 129]
                        O.mm((bkN[h // 2], o_ap), (mqd, mqd.t[:, h, r0:r1]), (Cb_, Cb_.t[:, h, 0:129]), start=True, stop=False)
                        O.mm((bkN[h // 2], o_ap), (pT, pT.t[r0:r1, h, r0:r1]), (mvx, mvx.t[r0:r1, h, 0:129]), start=False, stop=True)
                    dmax = (gsm2, gsm2.t[r0:r1, 0:4])
                    for q in range(2):
                        nv = bkN[q].t[r0:r1, 0:260].rearrange("p (h s) -> p h s", h=2)
                        dq = (gsm2, gsm2.t[r0:r1, 2 * q:2 * q + 2])
                        O.act(dq, (bkN[q], nv[:, :, 128]), AF.Abs)
                        O.ts(dq, dq, 1.0, op0=ALU.max)
                    O.recip(dmax, dmax)
                    for q in range(2):
                        nv = bkN[q].t[r0:r1, 0:260].rearrange("p (h s) -> p h s", h=2)
                        O.tt((o_sb, o_sb.t[r0:r1, 4 + 2 * q:6 + 2 * q, :]), (bkN[q], nv[:, :, 0:128]),
                             (gsm2, bc_last(gsm2.t[r0:r1, 2 * q:2 * q + 2], 128)), ALU.mult)
                bkC = [malloc_(), malloc_()]
                for h in range(4):
                    O.mm((bkC[h // 2], bkC[h // 2].t[:, (h % 2) * 130:(h % 2) * 130 + 129]), (kw, kw.t[r0:r1, h, :]),
                         (mvx, mvx.t[r0:r1, h, 0:129]))
                O.tt(V(C_), V(C_), (tot_b, bc_last(tot_b.t[:, 4 + 8 * blk:8 + 8 * blk], 130)), ALU.mult)
                for q in range(2):
                    O.tt((C_, C_.t[:, 2 * q:2 * q + 2, 0:129]), (C_, C_.t[:, 2 * q:2 * q + 2, 0:129]),
                         (bkC[q], bkC[q].t[:, 0:260].rearrange("p (h s) -> p h s", h=2)[:, :, 0:129]), ALU.add)
                O.cp((Cb_, Cb_.t[:, :, 0:129]), (C_, C_.t[:, :, 0:129]), eng="act")
            Mx.append(m_blk)
        g_pre = [s_ for s_ in G if s_.__name__ != "g_blk"]
        g_rec = [s_ for s_ in G if s_.__name__ == "g_blk"]
        m_pre = [s_ for s_ in Mx if s_.__name__ != "m_blk"]
        m_rec = [s_ for s_ in Mx if s_.__name__ == "m_blk"]
        Pl = list(g_pre)
        pos = 8 if with_out else 4
        for s_ in m_pre:
            Pl.insert(min(pos, len(Pl) - 1), s_)
            pos += 2
        B = []
        if first:
            B.append(reset_states)
        for s1, s2 in zip(g_rec, m_rec):
            B.append(s1); B.append(s2)
        own_i = ti - NT_OTHER
        if with_out and not final:
            def o_store():
                O.dma((ob_bufs[own_i], ob_d[own_i].rearrange("p (h d) -> p h d", h=8)), (o_sb, o_sb.t[:, :, :]))
            B.append(o_store)
        if final:
            def o_fin1():
                O.dma((ob_sb, ob_sb.t[:, :, :]), (ob_bufs[own_i], ob_d[own_i].rearrange("p (h d) -> p h d", h=8)))
                O.dma((xtok, xtok.t[:, :]), xown_d[own_i * 128:(own_i + 1) * 128, :])
                O.tt(V(o_sb), V(o_sb), V(ob_sb), ALU.add)
                O.tt(V(ob_sb), V(o_sb), V(o_sb), ALU.mult)
                ssq = (gsm2, gsm2.t[:, 8:16])
                O.red(ssq, V(ob_sb), ALU.add)
                O.act(ssq, ssq, AF.Ln, scale=1.0 / 128, bias=EPS)
                O.act(ssq, ssq, AF.Exp, scale=-0.5)
                O.tt(V(o_sb), V(o_sb), (gsm2, bc_last(gsm2.t[:, 8:16], 128)), ALU.mult)
                O.tt((o_sb, o_sb.t[:, 0:4, :]), (o_sb, o_sb.t[:, 0:4, :]), V(gng), ALU.mult)
                O.tt((o_sb, o_sb.t[:, 4:8, :]), (o_sb, o_sb.t[:, 4:8, :]), V(mng), ALU.mult)
                O.tt(V(y_sb), V(o_sb), V(zs), ALU.mult)

            def o_fin2():
                bk = malloc_()
                bkb = bk.t.bitcast(BF16)
                for c8 in range(8):
                    O.tr((bk, bkb[:, c8 * 128:(c8 + 1) * 128]), (y_sb, y_sb.t[:, c8, :]), (identb, identb.t[:, :]))
                O.cp(V(yT), (bk, bkb[:, :].rearrange("p (c t) -> p c t", c=8)), eng="act")

            def o_fin3():
                for half in range(2):
                    bk = malloc_()
                    for j in range(8):
                        O.mm((bk, bk.t[:, :]), (yT, yT.t[:, j, :]), (wout, wout.t[:, j, half * 512:(half + 1) * 512]),
                             start=(j == 0), stop=(j == 7))
                    O.tt((x1, x1.t[:, half * 512:(half + 1) * 512]), (xtok, xtok.t[:, half * 512:(half + 1) * 512]),
                         (bk, bk.t[:, :]), ALU.add)
                O.dma((x1_bufs[own_i], x1_d[own_i]), (x1, x1.t[:, :]))
            B += [o_fin1, o_fin2, o_fin3]
        return Pl, B

    sched = [(ti, 1, True, False) for ti in range(NT - 1, NT_OTHER - 1, -1)]
    sched += [(ti, 0, False, False) for ti in range(0, NT_OTHER)]
    sched += [(ti, 0, True, True) for ti in range(NT_OTHER, NT)]
    conv_i = 0
    nS = len(sched)
    pending = {}
    for j in range(-2, nS):
        if phase2 and j >= 1:
            left_tiles = nS - j
            todo = -(-(n_conv - conv_i) // max(1, left_tiles - 2)) if left_tiles > 2 else n_conv - conv_i
            for _ in range(min(todo, n_conv - conv_i)):
                conv_chunk(conv_i); conv_i += 1
        lists = []
        if 0 <= j < nS:
            lists.append(pending.pop(j))
        if 0 <= j + 1 < nS:
            Pl, Bl = back_steps(*sched[j + 1], j + 1, (j + 1) == 0 or (j + 1) == NT_OWN)
            pending[j + 1] = Bl
            lists.append(Pl)
        if 0 <= j + 2 < nS:
            lists.append(front_steps(*sched[j + 2], j + 2))
        idx = [0] * len(lists)
        tot_n = max([len(l) for l in lists] + [1])
        for step in range(tot_n):
            for li, l in enumerate(lists):
                want = ((step + 1) * len(l) + tot_n - 1) // tot_n
                while idx[li] < min(want, len(l)):
                    l[idx[li]](); idx[li] += 1
    assert (not phase2) or conv_i == n_conv
    if not phase2:
        for i in range(NT_OWN):
            O.dma((xtok, xtok.t[:, :]), (x1_bufs[i], x1_d[i]))
            O.dma((out_bufs[i], out_d[i * 128:(i + 1) * 128, :]), (xtok, xtok.t[:, :]))
    else:
        P.barrier()
        P.release(PH_MARK)
        phase2_build(nc, P, O, C, identb, iot, banks, NT_OWN, x1_bufs, x1_d, out_bufs, out_d,
                     wq_d, keysT_d, n2g_d, nfg_d, UTs, Vs, UTs_d, Vs_d)
    P.wait_all("sp", out_bufs)
    P.emit()
    return nc


def phase2_build(nc, P, O, C, identb, iot, banks, NT_OWN, x1_bufs, x1_d, out_bufs, out_d,
                 wq_d, keysT_d, n2g_d, nfg_d, UTs, Vs, UTs_d, Vs_d):
    I_f = C["ident"]
    TB = 256
    NB = NT_OWN // 2
    IG = 2
    TS = 8
    obank = [[banks[0], banks[1]], [banks[2], banks[3]]]
    pools = {"loop": [banks[4], banks[5], banks[6]], "prep": [banks[7]]}
    pidx = {"loop": 0, "prep": 0}

    def pbank(who):
        b = pools[who][pidx[who] % len(pools[who])]
        pidx[who] += 1
        return b

    def pv4(b):
        return b.t[:, :].rearrange("p (h s) -> p h s", h=4)

    def V(b, ap=None):
        return (b, b.t if ap is None else ap)

    wq = P.sb("wq", [128, 8, 2048], BF16)
    for j in range(8):
        O.dma((wq, wq.t[:, j, :]), wq_d[j * 128:(j + 1) * 128, :], eng="pool")
    keysT = P.sb("keysT", [128, 16, 128], F32)
    O.dma((keysT, keysT.t.rearrange("p a b -> p (a b)")), keysT_d)
    n2g = P.sb("n2g", [128, 1024], F32)
    O.dma(V(n2g), n2g_d)
    nfg = P.sb("nfg", [128, 1024], F32)
    O.dma(V(nfg), nfg_d)
    x1p = [P.sb("x1p0", [128, 1024], F32)] * 2
    xr = [P.sb("xr0", [128, 1024], F32)] * 2
    h2 = P.sb("h2", [128, 8, 128], BF16)
    h2T = [P.sb("h2T%d" % i, [128, 8, TB], BF16) for i in range(2)]
    qT = P.sb("qT", [128, 16, 128], F32)
    cand = (qT, qT.t.rearrange("p a b -> p (a b)").rearrange("p (h j r) -> p h j r", h=8, j=16))
    sc = P.sb("sc", [128, 16, 128], F32)
    oh = (sc, sc.t.rearrange("p a b -> p (a b)").rearrange("p (h j r) -> p h j r", h=8, j=16))
    top = P.sb("top", [128, 16, 16], F32)
    ti = P.sb("ti", [128, 16, 16], U32)
    tif = P.sb("tif", [128, 16, 16], F32)
    scr = P.sb("scr", [128, 256], F32)
    best = P.sb("best", [128, 8, 16], F32)
    pos = P.sb("pos", [128, 8, 16], U32)
    rr = P.sb("rr", [128, 2, 8, 16], U32)
    rrf = P.sb("rrf", [128, 2, 8, 16], F32)
    abw = P.sb("abw", [128, 3, 128], F32)
    abwT = [P.sb("abwT%d" % i, [128, 3, TB], F32) for i in range(2)]
    abT16 = [P.sb("abT16_%d" % i, [128, 2, TB], BF16) for i in range(2)]
    iot16 = P.sb("iot16", [128, 128], BF16)
    O.cp((iot16, iot16.t), (iot, iot.t[:, 16:144]), eng="dve")
    sm = P.sb("p2sm", [128, 64], F32)
    smf = P.sb("p2smf", [128, 8], F32)
    Acol = [P.sb("Acol%d" % i, [128, TS, 128], BF16) for i in range(2)]
    Bcol = [P.sb("Bcol%d" % i, [128, TS, 128], BF16) for i in range(2)]
    Gs = P.sb("Gs", [128, TB, 128], BF16)
    UTb = [P.sb("UTb%d" % i, [128, IG, 8, 128], BF16) for i in range(3)]
    Vb = [P.sb("Vb%d" % i, [128, IG, 1024], BF16) for i in range(3)]
    actb = [P.sb("actb%d" % i, [128, TB], BF16) for i in range(3)]
    GA = [P.sb("GA%d" % i, [128, TB], BF16) for i in range(3)]
    topv = top.t.rearrange("p (h q) r -> p h q r", q=2)
    tifv = tif.t.rearrange("p (h q) r -> p h q r", q=2)
    candh = (qT, cand[1][:, 0, 0:8, :])
    sqscr = (sc, sc.t.rearrange("p a b -> p (a b)")[:, 0:1024])
    sqscr2 = None

    def prep_steps(blk, pb):
        st = []
        hT_ = h2T[pb]; aT_ = abwT[pb]; a16_ = abT16[pb]
        for tt in range(2):
            tix = blk * 2 + tt
            xb = x1p[tt]
            tsl = slice(tt * 128, (tt + 1) * 128)

            def s_norm(tix=tix, xb=xb, tt=tt):
                O.dma(V(xb), (x1_bufs[tix], x1_d[tix]))
                O.act(sqscr, V(xb), AF.Square, accum=(sm, sm.t[:, 0:1]))
                O.act((sm, sm.t[:, 0:1]), (sm, sm.t[:, 0:1]), AF.Sqrt, scale=1.0 / 1024, bias=EPS)
                O.recip((sm, sm.t[:, 0:1]), (sm, sm.t[:, 0:1]))
                O.stt((h2, h2.t.rearrange("p a b -> p (a b)")), V(xb), (sm, sm.t[:, 0:1]), V(n2g), ALU.mult, ALU.mult)
            st.append(s_norm)

            def s_normT(tt=tt):
                bk = pbank("prep")
                bkb = bk.t.bitcast(BF16)
                for c8 in range(8):
                    O.tr((bk, bkb[:, c8 * 128:(c8 + 1) * 128]), (h2, h2.t[:, c8, :]), V(identb))
                O.cp((hT_, hT_.t[:, :, tt * 128:(tt + 1) * 128]), (bk, bkb[:, :].rearrange("p (c t) -> p c t", c=8)), eng="act")
            st.append(s_normT)
            for g4 in range(4):
                def s_q(g4=g4, tsl=tsl):
                    bk = pbank("prep")
                    for q in range(4):
                        hp = g4 * 4 + q
                        for j in range(8):
                            O.mm((bk, pv4(bk)[:, q, :]), (wq, wq.t[:, j, hp * 128:(hp + 1) * 128]), (hT_, hT_.t[:, j, tsl]),
                                 start=(j == 0), stop=(j == 7))
                    O.cp((qT, qT.t[:, g4 * 4:(g4 + 1) * 4, :]), (bk, pv4(bk)), eng="act")
                st.append(s_q)
            for g4 in range(4):
                def s_sc(g4=g4):
                    bk = pbank("prep")
                    for q in range(4):
                        hp = g4 * 4 + q
                        O.mm((bk, pv4(bk)[:, q, :]), (qT, qT.t[:, hp, :]), (keysT, keysT.t[:, hp, :]))
                    O.cp((sc, sc.t[:, g4 * 4:(g4 + 1) * 4, :]), (bk, pv4(bk)), eng="act")
                st.append(s_sc)
            for hp in range(16):
                def s_top(hp=hp):
                    s_hp = sc.t[:, hp, :]
                    t8a = top.t[:, hp, 0:8]; t8b = top.t[:, hp, 8:16]
                    i8a = ti.t[:, hp, 0:8]; i8b = ti.t[:, hp, 8:16]
                    s128 = scr.t[:, 0:128]
                    P.op("dve", lambda e: e.max(out=t8a, in_=s_hp), reads=[sc], writes=[top])
                    P.op("dve", lambda e: e.max_index(out=i8a, in_max=t8a, in_values=s_hp), reads=[sc, top], writes=[ti])
                    P.op("dve", lambda e: e.match_replace(out=s128, in_to_replace=t8a, in_values=s_hp, imm_value=-1e30), reads=[sc, top], writes=[scr])
                    P.op("dve", lambda e: e.max(out=t8b, in_=s128), reads=[scr], writes=[top])
                    P.op("dve", lambda e: e.max_index(out=i8b, in_max=t8b, in_values=s128), reads=[scr, top], writes=[ti])
                st.append(s_top)

            def s_cand():
                O.cp(V(tif), V(ti), eng="dve")
                O.tt(cand, (top, topv[:, :, 0, :].unsqueeze(3).to_broadcast([128, 8, 16, 16])),
                     (top, topv[:, :, 1, :].unsqueeze(2).to_broadcast([128, 8, 16, 16])), ALU.add)
            st.append(s_cand)
            for h in range(8):
                def s_best(h=h):
                    c_h = cand[1][:, h, :, :].rearrange("p a b -> p (a b)")
                    b8a = best.t[:, h, 0:8]; b8b = best.t[:, h, 8:16]
                    p8a = pos.t[:, h, 0:8]; p8b = pos.t[:, h, 8:16]
                    s256 = scr.t[:, :]
                    P.op("dve", lambda e: e.max(out=b8a, in_=c_h), reads=[qT], writes=[best])
                    P.op("dve", lambda e: e.max_index(out=p8a, in_max=b8a, in_values=c_h), reads=[qT, best], writes=[pos])
                    P.op("dve", lambda e: e.match_replace(out=s256, in_to_replace=b8a, in_values=c_h, imm_value=-1e30), reads=[qT, best], writes=[scr])
                    P.op("dve", lambda e: e.max(out=b8b, in_=s256), reads=[scr], writes=[best])
                    P.op("dve", lambda e: e.max_index(out=p8b, in_max=b8b, in_values=s256), reads=[scr, best], writes=[pos])
                st.append(s_best)

            def s_idx():
                P.op("dve", lambda e: e.tensor_single_scalar(out=rr.t[:, 0, :, :], in_=pos.t, scalar=4, op=ALU.logical_shift_right), reads=[pos], writes=[rr])
                P.op("dve", lambda e: e.tensor_single_scalar(out=rr.t[:, 1, :, :], in_=pos.t, scalar=15, op=ALU.bitwise_and), reads=[pos], writes=[rr])
                O.cp(V(rrf), V(rr), eng="dve")
            st.append(s_idx)
            for q in range(2):
                def s_lk(q=q):
                    O.tt(oh, (iot, iot.t[:, 0:16].unsqueeze(1).unsqueeze(1).to_broadcast([128, 8, 16, 16])),
                         (rrf, rrf.t[:, q, :, :].unsqueeze(3).to_broadcast([128, 8, 16, 16])), ALU.is_equal)
                    O.tt(oh, oh, (tif, tifv[:, :, q, :].unsqueeze(2).to_broadcast([128, 8, 16, 16])), ALU.mult)
                    O.red((abw, abw.t[:, q, :].rearrange("p (h j) -> p h j", h=8)), oh, ALU.add)
                st.append(s_lk)

            def s_gate(tsl=tsl):
                O.tt(candh, V(best), (best, bc_last(best.t[:, :, 0], 16)), ALU.subtract)
                O.act(candh, candh, AF.Exp)
                O.red((sm, sm.t[:, 8:16]), candh, ALU.add)
                O.recip((sm, sm.t[:, 8:16]), (sm, sm.t[:, 8:16]))
                O.tt((abw, abw.t[:, 2, :].rearrange("p (h j) -> p h j", h=8)), candh,
                     (sm, bc_last(sm.t[:, 8:16], 16)), ALU.mult)
            st.append(s_gate)

            def s_gateT(tsl=tsl):
                bk = pbank("prep")
                for q in range(3):
                    O.tr((bk, bk.t[:, q * 128:(q + 1) * 128]), (abw, abw.t[:, q, :]), I_f)
                O.cp((aT_, aT_.t[:, :, tsl]), (bk, bk.t[:, 0:384].rearrange("p (q t) -> p q t", q=3)), eng="act")
                O.cp((a16_, a16_.t[:, :, tsl]), (bk, bk.t[:, 0:256].rearrange("p (q t) -> p q t", q=2)), eng="act")
            st.append(s_gateT)
        return st

    def scatter(pb):
        aT_ = abwT[pb]; a16_ = abT16[pb]
        for sb_i in range(TB // TS):
            t0 = sb_i * TS
            Ac = Acol[sb_i % 2]; Bc = Bcol[sb_i % 2]
            io_bc = (iot16, iot16.t.unsqueeze(1).to_broadcast([128, TS, 128]))
            O.tt(V(Ac), io_bc, (a16_, bc_last(a16_.t[:, 0, t0:t0 + TS], 128)), ALU.is_equal)
            O.tt(V(Ac), V(Ac), (aT_, bc_last(aT_.t[:, 2, t0:t0 + TS], 128)), ALU.mult, eng="pool")
            O.tt(V(Bc), io_bc, (a16_, bc_last(a16_.t[:, 1, t0:t0 + TS], 128)), ALU.is_equal)
            for q4 in range(TS // 4):
                bk = pbank("loop" if (sb_i * (TS // 4) + q4) % 4 else "prep")
                for q in range(4):
                    tl = q4 * 4 + q
                    O.mm((bk, pv4(bk)[:, q, :]), (Ac, Ac.t[:, tl, :]), (Bc, Bc.t[:, tl, :]))
                O.cp((Gs, Gs.t[:, t0 + q4 * 4:t0 + q4 * 4 + 4, :]), (bk, pv4(bk)), eng="act")

    NG = 128 // IG

    def load_group(g):
        ub = UTb[g % 3]; vb_ = Vb[g % 3]
        O.dma((ub, ub.t.rearrange("p a b c -> p (a b c)")), (UTs, UTs_d[:, g * IG * 1024:(g + 1) * IG * 1024]), eng="sp")
        O.dma((vb_, vb_.t.rearrange("p a b -> p (a b)")), (Vs, Vs_d[:, g * IG * 1024:(g + 1) * IG * 1024]), eng="sp")

    def emit_v(ga, vb_, il, i2):
        for tt in range(2):
            for dh in range(2):
                O.mm((obank[tt][dh], obank[tt][dh].t[:, :]), (ga, ga.t[:, tt * 128:(tt + 1) * 128]),
                     (vb_, vb_.t[:, il, dh * 512:(dh + 1) * 512]), start=(i2 == 0), stop=(i2 == 127))

    def expert_loop(pb, side):
        hT_ = h2T[pb]
        per = (len(side) + 127) // 128 if side else 0
        si = 0
        load_group(0)
        load_group(1)
        pendq = []
        for g in range(NG):
            ub = UTb[g % 3]; vb_ = Vb[g % 3]
            for il in range(IG):
                i2 = g * IG + il
                bk = pbank("loop")
                for j in range(8):
                    O.mm((bk, bk.t[:, 0:TB]), (ub, ub.t[:, il, j, :]), (hT_, hT_.t[:, j, :]), start=(j == 0), stop=(j == 7))
                ab = actb[i2 % 3]; ga = GA[i2 % 3]
                O.act(V(ab), (bk, bk.t[:, 0:TB]), AF.Gelu)
                O.tt(V(ga), V(ab), (Gs, Gs.t[:, :, i2]), ALU.mult, eng="pool")
                pendq.append((ga, vb_, il, i2))
                if len(pendq) > 2:
                    emit_v(*pendq.pop(0))
                if il == 1 and g + 2 < NG:
                    load_group(g + 2)
                for _ in range(per):
                    if si < len(side):
                        side[si](); si += 1
        while pendq:
            emit_v(*pendq.pop(0))
        while si < len(side):
            side[si](); si += 1

    def final(blk):
        for tt in range(2):
            tix = blk * 2 + tt
            xb = xr[tt]
            O.dma(V(xb), (x1_bufs[tix], x1_d[tix]))
            for dh in range(2):
                O.tt((xb, xb.t[:, dh * 512:(dh + 1) * 512]), (xb, xb.t[:, dh * 512:(dh + 1) * 512]),
                     (obank[tt][dh], obank[tt][dh].t[:, :]), ALU.add)
            O.act((Gs, Gs.t[:, 0:8, :].rearrange("p a b -> p (a b)")), V(xb), AF.Square, accum=(smf, smf.t[:, tt:tt + 1]))
            O.act((smf, smf.t[:, tt:tt + 1]), (smf, smf.t[:, tt:tt + 1]), AF.Sqrt, scale=1.0 / 1024, bias=EPS)
            O.recip((smf, smf.t[:, tt:tt + 1]), (smf, smf.t[:, tt:tt + 1]))
            O.stt(V(xb), V(xb), (smf, smf.t[:, tt:tt + 1]), V(nfg), ALU.mult, ALU.mult)
            O.dma((out_bufs[tix], out_d[tix * 128:(tix + 1) * 128, :]), V(xb))

    for s_ in prep_steps(0, 0):
        s_()
    for blk in range(NB):
        pb = blk % 2
        scatter(pb)
        side = prep_steps(blk + 1, 1 - pb) if blk + 1 < NB else []
        expert_loop(pb, side)
        final(blk)


def prep_shared(inp):
    f32 = np.float32

    def rep(v):
        return np.repeat(np.asarray(v, f32).reshape(1, -1), 128, axis=0)
    d = {}
    keys = np.asarray(inp["peer_keys"], f32)[0]
    d["keysT"] = keys.transpose(3, 0, 1, 2).reshape(128, 2048)
    d["wq"] = np.asarray(inp["peer_wq"], f32)[0]
    d["n2g"] = rep(np.asarray(inp["norm2_g"], f32)[0])
    d["nfg"] = rep(np.asarray(inp["normf_g"], f32))
    U = np.asarray(inp["peer_u"], f32)[0]
    d["UT"] = U.reshape(128, 128, 8, 128).transpose(3, 1, 2, 0).reshape(128, 128 * 1024)
    d["Vt"] = np.asarray(inp["peer_v"], f32)[0].reshape(128, 128 * 1024)
    d["iotas"] = np.concatenate([rep(np.arange(16)), rep(np.arange(128))], axis=1)
    d["w_out"] = np.asarray(inp["w_out"], f32)[0]
    d["consts"] = CONST_ARR
    d["gdn_norm_g"] = rep(np.asarray(inp["gdn_norm_g"], f32)[0])
    d["mlstm_norm_g"] = rep(np.asarray(inp["mlstm_norm_g"], f32)[0])
    return {k: np.ascontiguousarray(v, dtype=f32) for k, v in d.items()}


def prep_core(inp, b, half, S, shared=None):
    f32 = np.float32
    x = np.asarray(inp["x"], f32)[b][:S]
    xl = x[::-1] if half == 0 else x
    df, db = (0, 1) if half == 1 else (1, 0)
    S_OWN = S // 2
    xT = np.zeros((1024, S + 4), f32)
    xT[:, 2:S + 2] = xl.T
    w_in = np.asarray(inp["w_in"], f32)[0]
    sizes = [512, 512, 512, 512, 8, 8, 512, 512, 512, 512, 8, 8]
    offs = np.cumsum([0] + sizes)
    gq, gk, gv, gz, ga, gb, mq, mk, mv, mo, mi, mf = [w_in[:, offs[i]:offs[i + 1]] for i in range(12)]

    def dsel(w):
        return np.concatenate([w[:, 4 * df:4 * df + 4], w[:, 4 * db:4 * db + 4]], axis=1)
    gates = np.concatenate([dsel(ga), dsel(gb), dsel(mi), dsel(mf)], axis=1)
    w_fm = np.concatenate([gq, gk, gv, mq], axis=1)
    w_tm = np.concatenate([gz, mo, mv, mk, gates], axis=1)
    cw = np.asarray(inp["conv_w"], f32)[0]
    if half == 0:
        cw = cw[:, ::-1]
    convw = cw.reshape(12, 128, 5).transpose(1, 0, 2).reshape(128, 60)
    n1g = np.asarray(inp["norm1_g"], f32)[0].reshape(8, 128).T

    def rep(v):
        return np.repeat(np.asarray(v, f32).reshape(1, -1), 128, axis=0)

    def dvec(p):
        p = np.asarray(p, f32)[0]
        return np.concatenate([p[df], p[db]])
    gp_add = np.concatenate([dvec(inp["gdn_dt_bias"]), np.zeros(8, f32), dvec(inp["mlstm_i_bias"]), dvec(inp["mlstm_f_bias"])])
    d = dict(
        xT=xT, x_own=np.ascontiguousarray(xl[S - S_OWN:]),
        w_in_fm=np.ascontiguousarray(w_fm), w_in_tm=np.ascontiguousarray(w_tm),
        conv_w=np.ascontiguousarray(convw),
        norm1_g=np.ascontiguousarray(n1g), gp_add=rep(gp_add), alog=rep(dvec(inp["gdn_a_log"])),
    )
    if shared is None:
        shared = prep_shared(inp)
    d = {k: np.ascontiguousarray(v, dtype=f32) for k, v in d.items()}
    d.update(shared)
    return d


_NC_CACHE = {}


def kernel(**inputs):
    from concourse.bass_utils import run_bass_kernel_spmd
    x = np.asarray(inputs["x"])
    B, S, D = x.shape
    NTH = S // 256
    key = (NTH,)
    if key not in _NC_CACHE:
        _NC_CACHE[key] = build(NTH, NTH, phase2=True)
    nc = _NC_CACHE[key]
    shared = prep_shared(inputs)
    maps = []
    for c in range(8):
        maps.append(prep_core(inputs, c // 2, c % 2, S, shared))
    res = run_bass_kernel_spmd(nc, maps, core_ids=list(range(8)))
    out = np.empty((B, S, D), np.float32)
    for c in range(8):
        b, half = c // 2, c % 2
        o = res.results[c]["out"]
        if half == 0:
            out[b, :S // 2] = o[::-1]
        else:
            out[b, S // 2:] = o
    return out
```
